# Optimizing a Trainium2 kernel written in Bass

```python
import jax, jax.numpy as jnp
from jax import lax
import numpy as np

D_MODEL = 1024
BATCH = 4
SEQ = 8192
DEPTH = 4

MEM_LEN = 256
RET_HEADS = 4
RET_QK_DIM = 128
RET_V_DIM = 128
RET_WIDTH = RET_HEADS * RET_QK_DIM
RET_CHUNK = 128
ROPE_BASE = 10000.0
POOL_WINDOWS = (2, 4, 8, 16)
POOL_GROUPS = 4
POOL_GROUP_DIM = D_MODEL // 16
POOL_WIDTH = POOL_GROUPS * POOL_GROUP_DIM
XA_HEADS = 4
XA_HEAD_DIM = D_MODEL // 16
XA_WIDTH = XA_HEADS * XA_HEAD_DIM
N_BRANCH = 3
IN_SPLITS = (RET_WIDTH, RET_WIDTH, RET_HEADS * RET_V_DIM, RET_HEADS * RET_V_DIM, POOL_WIDTH, XA_WIDTH, N_BRANCH * D_MODEL)
IN_WIDTH = sum(IN_SPLITS)
FFN_HIDDEN = -(-8 * D_MODEL // (3 * 256)) * 256
EPS = 1e-6

kernel_name = "hybrid_retention_pool_memxattn_block"


def rmsnorm(x, g):
    xf = x.astype(jnp.float32)
    y = xf * lax.rsqrt(jnp.mean(xf * xf, axis=-1, keepdims=True) + EPS)
    return (y * g.astype(jnp.float32)).astype(x.dtype)


def rope(t, cos, sin):
    t1, t2 = jnp.split(t, 2, axis=-1)
    return jnp.concatenate([t1 * cos - t2 * sin, t2 * cos + t1 * sin], axis=-1)


def retention_chunkwise(q, k, v):
    B, S, H, Dk = q.shape
    Dv = v.shape[-1]
    C = RET_CHUNK
    N = S // C
    q = q.reshape(B, N, C, H, Dk)
    k = k.reshape(B, N, C, H, Dk)
    v = v.reshape(B, N, C, H, Dv)
    log_g = jnp.log(1.0 - jnp.exp2(-5.0 - jnp.arange(H, dtype=jnp.float32)))
    idx = jnp.arange(C, dtype=jnp.float32)
    diff = idx[:, None] - idx[None, :]
    decay_in = jnp.where(diff[None] >= 0, jnp.exp(jnp.maximum(diff, 0.0)[None] * log_g[:, None, None]), 0.0)
    scores = jnp.einsum('bnihd,bnjhd->bnhij', q, k) * decay_in[None, None]
    inner = jnp.einsum('bnhij,bnjhe->bnihe', scores, v)
    zeta = jnp.exp((C - 1.0 - idx)[None, :] * log_g[:, None])
    chunk_kv = jnp.einsum('bnjhd,bnjhe,hj->bnhde', k, v, zeta)
    g_chunk = jnp.exp(C * log_g)[None, :, None, None]

    def step(R, kv):
        return R * g_chunk + kv, R

    _, R_prev = lax.scan(step, jnp.zeros((B, H, Dk, Dv), jnp.float32), jnp.moveaxis(chunk_kv, 1, 0))
    R_prev = jnp.moveaxis(R_prev, 0, 1)
    xi = jnp.exp((idx + 1.0)[None, :] * log_g[:, None])
    cross = jnp.einsum('bnihd,bnhde,hi->bnihe', q, R_prev, xi)
    return (inner + cross).reshape(B, S, H, Dv)


def multiscale_pool(u):
    B, S, _ = u.shape
    uf = u.astype(jnp.float32).reshape(B, S, POOL_GROUPS, POOL_GROUP_DIM)
    cpad = jnp.pad(jnp.cumsum(uf, axis=1), ((0, 0), (1, 0), (0, 0), (0, 0)))
    t = jnp.arange(S)
    outs = []
    for g, w in enumerate(POOL_WINDOWS):
        c = cpad[:, :, g]
        lagged = jnp.pad(c[:, :S + 1 - w], ((0, 0), (w - 1, 0), (0, 0)))
        count = jnp.minimum(t + 1, w).astype(jnp.float32)[None, :, None]
        outs.append((c[:, 1:] - lagged) / count - uf[:, :, g])
    return jnp.stack(outs, axis=2)


def memory_xattn(q, mem, g_mem, w_mem_kv):
    B, S, _ = q.shape
    M = mem.shape[1]
    kv = rmsnorm(mem, g_mem) @ w_mem_kv
    mk, mv = jnp.split(kv, 2, axis=-1)
    qh = q.reshape(B, S, XA_HEADS, XA_HEAD_DIM)
    mk = mk.reshape(B, M, XA_HEADS, XA_HEAD_DIM)
    mv = mv.reshape(B, M, XA_HEADS, XA_HEAD_DIM)
    s = jnp.einsum('bshd,bmhd->bhsm', qh, mk).astype(jnp.float32) * (XA_HEAD_DIM ** -0.5)
    p = jax.nn.softmax(s, axis=-1).astype(mv.dtype)
    return jnp.einsum('bhsm,bmhd->bshd', p, mv).reshape(B, S, XA_WIDTH)


def setup_inputs(seed: int = 0) -> dict:
    key = jax.random.key(seed)
    ks = jax.random.split(key, 20)
    nrm = lambda k, shape, fan: jax.random.normal(k, shape, jnp.float32) * (fan ** -0.5)
    gain = lambda k, shape: 1.0 + 0.02 * jax.random.normal(k, shape, jnp.float32)
    return {
        "x": jax.random.normal(ks[0], (BATCH, SEQ, D_MODEL), jnp.float32),
        "mem": jax.random.normal(ks[1], (BATCH, MEM_LEN, D_MODEL), jnp.float32),
        "positions": jnp.arange(SEQ, dtype=jnp.int32),
        "norm_mix": gain(ks[2], (DEPTH, D_MODEL)),
        "w_in": nrm(ks[3], (DEPTH, D_MODEL, IN_WIDTH), D_MODEL),
        "w_up_ret": nrm(ks[4], (DEPTH, RET_HEADS * RET_V_DIM, D_MODEL), RET_HEADS * RET_V_DIM),
        "w_pool_mix": nrm(ks[5], (DEPTH, POOL_GROUPS, POOL_GROUP_DIM, POOL_GROUP_DIM), POOL_GROUP_DIM),
        "pool_scale": 1.0 + 0.1 * jax.random.normal(ks[6], (DEPTH, POOL_WIDTH), jnp.float32),
        "w_up_pool": nrm(ks[7], (DEPTH, POOL_WIDTH, D_MODEL), POOL_WIDTH),
        "norm_mem": gain(ks[8], (DEPTH, D_MODEL)),
        "w_mem_kv": nrm(ks[9], (DEPTH, D_MODEL, 2 * XA_WIDTH), D_MODEL),
        "w_up_x": nrm(ks[10], (DEPTH, XA_WIDTH, D_MODEL), XA_WIDTH),
        "w_out": nrm(ks[11], (DEPTH, D_MODEL, D_MODEL), D_MODEL),
        "norm_ffn": gain(ks[12], (DEPTH, D_MODEL)),
        "w_ffn_in": nrm(ks[13], (DEPTH, D_MODEL, 2 * FFN_HIDDEN), D_MODEL),
        "w_ffn_out": nrm(ks[14], (DEPTH, FFN_HIDDEN, D_MODEL), FFN_HIDDEN),
        "final_norm": gain(ks[15], (D_MODEL,)),
    }


def reference(x, mem, positions, norm_mix, w_in, w_up_ret, w_pool_mix, pool_scale, w_up_pool,
              norm_mem, w_mem_kv, w_up_x, w_out, norm_ffn, w_ffn_in, w_ffn_out, final_norm):
    B, S, D = x.shape
    inv_freq = ROPE_BASE ** (-jnp.arange(0, RET_QK_DIM, 2, dtype=jnp.float32) / RET_QK_DIM)
    ang = positions.astype(jnp.float32)[:, None] * inv_freq[None, :]
    cos = jnp.cos(ang)[None, :, None, :]
    sin = jnp.sin(ang)[None, :, None, :]
    split_pts = np.cumsum(IN_SPLITS)[:-1].tolist()

    for l in range(DEPTH):
        h = rmsnorm(x, norm_mix[l])
        z = h @ w_in[l]
        q, k, v, g_ret, u_pool, q_x, gates = jnp.split(z, split_pts, axis=-1)

        qr = rope(q.reshape(B, S, RET_HEADS, RET_QK_DIM).astype(jnp.float32), cos, sin)
        kr = rope(k.reshape(B, S, RET_HEADS, RET_QK_DIM).astype(jnp.float32), cos, sin) * (RET_QK_DIM ** -0.5)
        vr = v.reshape(B, S, RET_HEADS, RET_V_DIM).astype(jnp.float32)
        y = retention_chunkwise(qr, kr, vr)
        y = y * lax.rsqrt(jnp.mean(y * y, axis=-1, keepdims=True) + EPS)
        y_ret = (y.reshape(B, S, RET_HEADS * RET_V_DIM) * jax.nn.silu(g_ret.astype(jnp.float32))).astype(x.dtype)
        b_ret = y_ret @ w_up_ret[l]

        pooled = multiscale_pool(u_pool)
        mixed = jnp.einsum('bsgc,gcd->bsgd', pooled, w_pool_mix[l].astype(jnp.float32))
        y_pool = (mixed.reshape(B, S, POOL_WIDTH) * pool_scale[l].astype(jnp.float32)).astype(x.dtype)
        b_pool = y_pool @ w_up_pool[l]

        b_x = memory_xattn(q_x, mem, norm_mem[l], w_mem_kv[l]) @ w_up_x[l]

        gt = jax.nn.sigmoid(gates.reshape(B, S, N_BRANCH, D))
        merged = gt[:, :, 0] * b_ret + gt[:, :, 1] * b_pool + gt[:, :, 2] * b_x
        x = x + merged @ w_out[l]

        h2 = rmsnorm(x, norm_ffn[l])
        a, bgate = jnp.split(h2 @ w_ffn_in[l], 2, axis=-1)
        x = x + (jax.nn.silu(a) * bgate) @ w_ffn_out[l]

    return rmsnorm(x, final_norm)
```

```python
import contextlib
import numpy as np
import concourse.bass as bass
import concourse.mybir as mybir
from concourse.bass_utils import run_bass_kernel_spmd

F32, BF16, I32 = mybir.dt.float32, mybir.dt.bfloat16, mybir.dt.int32
ALU = mybir.AluOpType
AF = mybir.ActivationFunctionType

D = 1024
KC = 8
T = 512
NCH = T // 128
H = 4
MEM = 256
FH = 2816
FKC = 22
IN_W = 5632
EPS = 1e-6
NB = 4
SLABW = 4096

SLABS = [("memkv", 4096), ("q", 4096), ("k", 4096), ("v", 4096), ("gr", 4096), ("ux", 4096)]
SLABS += [(f"mg{c}", 4096) for c in range(8)]
SLABS += [("wo0", 4096), ("wo1", 4096)]
SLABS += [(f"fi{s}", 4096) for s in range(11)]
SLABS += [(f"fo{j}", 2816) for j in range(8)]
SLAB_OFF = {}
_o = 0
for _n, _w in SLABS:
    SLAB_OFF[_n] = (_o, _w)
    _o += _w
LINE = _o

C_MASK = 0
C_KSC = 512
C_GC = 1024
C_EPSR = 1536
C_INVW = 2048
C_CORR = 2050
C_INVF = 2082
C_SGN = 2083
C_EPS = 2084
C_ZERO = 2085
NCONST = 2088
B_ID = 0
B_PERM = 128
B_ONES = 256
B_OPAD = 384
NCB = 640


class Op:
    __slots__ = ("eng", "fn", "waits", "idx", "needed", "dma", "val")


class Sched:
    ENGS = ("pe", "act", "dve", "pool", "sp")

    def __init__(self):
        self.streams = {e: [] for e in self.ENGS}
        self.last_w = {}
        self.readers = {}
        self.known = {e: {} for e in self.ENGS}
        self.dma_count = {}

    def add(self, eng, fn, reads=(), writes=(), dma=None):
        op = Op()
        op.eng, op.fn, op.needed, op.dma, op.val = eng, fn, False, dma, None
        st = self.streams[eng]
        op.idx = len(st)
        deps = []
        for p in reads:
            w = self.last_w.get(p)
            if w is not None:
                deps.append((w, True))
            if p[0] in ("PS", "PT"):
                rd = self.readers.get(p)
                if rd:
                    for k_, r in rd.items():
                        if k_ != eng:
                            deps.append((r, True))
        for p in writes:
            w = self.last_w.get(p)
            if w is not None:
                deps.append((w, True))
            rd = self.readers.get(p)
            if rd:
                for r in rd.values():
                    deps.append((r, False))
        waits = {}
        kn = self.known[eng]
        for d, hazard_rw in deps:
            if d is op:
                continue
            if d.dma is not None:
                key = ("dma", id(d.dma))
                if kn.get(key, 0) >= d.val:
                    continue
                kn[key] = d.val
                waits[key] = d
                continue
            if d.eng == eng:
                if eng == "pe" or eng == "sp":
                    continue
                if not hazard_rw:
                    continue
            key = d.eng
            if kn.get(key, -1) >= d.idx:
                continue
            kn[key] = d.idx
            d.needed = True
            prev = waits.get(key)
            if prev is None or prev.idx < d.idx:
                waits[key] = d
        op.waits = list(waits.values())
        if dma is not None:
            c = self.dma_count.get(id(dma), 0) + 1
            self.dma_count[id(dma)] = c
            op.val = 16 * c
        st.append(op)
        for p in reads:
            self.readers.setdefault(p, {})[eng if dma is None else ("dma", id(op))] = op
        for p in writes:
            self.last_w[p] = op
            self.readers[p] = {}
        return op

    def mark_written(self, parts, op):
        for p in parts:
            self.last_w[p] = op
            self.readers[p] = {}

    def emit(self, eng, e, esem):
        if not hasattr(self, "_vals"):
            self._vals = True
            for en in self.ENGS:
                c = 0
                for op in self.streams[en]:
                    if op.dma is None and op.needed:
                        c += 1
                        op.val = c
        for op in self.streams[eng]:
            for d in op.waits:
                if d.dma is not None:
                    e.wait_ge(d.dma, d.val)
                else:
                    e.wait_ge(esem[d.eng], d.val)
            ins = op.fn(e)
            if op.dma is not None:
                ins.then_inc(op.dma, 16)
            elif op.needed:
                ins.then_inc(esem[eng], 1)


class _Stop(Exception):
    pass


def build_program(S, DEPTH, stop=None):
    NT = S // T
    stage_ctr = [0]
    stopped = [False]

    def stage(name):
        stage_ctr[0] += 1
        if stop is not None and stage_ctr[0] >= stop:
            if not stopped[0]:
                print('STOP at stage', stage_ctr[0], name)
            stopped[0] = True
    nc = bass.Bass("TRN2", target_bir_lowering=False)
    xT_d = nc.dram_tensor("xT", [D, S], F32, kind="ExternalInput").ap()
    memT_d = nc.dram_tensor("memT", [D, MEM], F32, kind="ExternalInput").ap()
    pos_d = nc.dram_tensor("pos", [1, S], I32, kind="ExternalInput").ap()
    wf_d = nc.dram_tensor("wf", [DEPTH, 128, LINE], F32, kind="ExternalInput").ap()
    cst_d = nc.dram_tensor("cst", [128, NCONST], F32, kind="ExternalInput").ap()
    cstb_d = nc.dram_tensor("cstb", [128, NCB], F32, kind="ExternalInput").ap()
    gains_d = nc.dram_tensor("gains", [128, DEPTH * 26 + 8], F32, kind="ExternalInput").ap()
    wmix_d = nc.dram_tensor("wmix", [128, DEPTH * 2 * 128], F32, kind="ExternalInput").ap()
    out_d = nc.dram_tensor("outT", [D, S], F32, kind="ExternalOutput").ap()
    wb_d = nc.dram_tensor("wb", [DEPTH, 128, LINE], BF16, kind="Internal").ap()

    es = contextlib.ExitStack()
    with es:
        def sb(name, shape, dt):
            return es.enter_context(nc.sbuf_tensor(name, shape, dt))

        def sem(name):
            return es.enter_context(nc.semaphore(name))

        X = sb("X", [128, KC, T], F32)
        Hh = sb("Hh", [128, KC, T], BF16)
        SQ = sb("SQ", [128, 2, T], BF16)
        RSTD = sb("RSTD", [128, T], F32)
        AR = sb("AR", [128, 22, T], BF16)
        RA = sb("RA", [128, T], F32)
        RB = sb("RB", [128, T], F32)
        QB = sb("QB", [128, 2, T], BF16)
        U = sb("U", [128, 2, T + 15], F32)
        PB = sb("PB", [128, 2, T + 15], F32)
        PC = sb("PC", [128, 2, T + 15], F32)
        POOLED = sb("POOLED", [128, 2, T], BF16)
        YPOOL = sb("YPOOL", [128, 2, T], BF16)
        EXPT = sb("EXPT", [128, 2, 2, T], BF16)
        YX = sb("YX", [128, 2, T], BF16)
        RDEN = sb("RDEN", [128, T], F32)
        ST = sb("ST", [128, 2, T], BF16)
        KTOK = sb("KTOK", [128, 2, T], BF16)
        YSQ = sb("YSQ", [128, 2, T], BF16)
        TT = sb("TT", [128, T], F32)
        HR = sb("HR", [128, T], F32)
        Y1 = sb("Y1", [128, T], F32)
        TG = sb("TG", [128, 6, T], F32)
        Mm = sb("Mm", [128, 6, T], F32)
        TH = sb("TH", [128, 2, T], F32)
        R32 = sb("R32", [128, DEPTH, T], F32)
        RBF = sb("RBF", [128, DEPTH, T], BF16)
        HALO = sb("HALO", [128, DEPTH, 2, 15], F32)
        MK = sb("MK", [128, DEPTH, 2, MEM], BF16)
        MV = sb("MV", [128, DEPTH, 2, 4, 128], BF16)
        WR = sb("WR", [128, NB, SLABW], BF16)
        COS = sb("COS", [128, T], F32)
        SIN = sb("SIN", [128, T], F32)
        POSI = sb("POSI", [128, T], I32)
        CST = sb("CST", [128, NCONST], F32)
        CB = sb("CB", [128, NCB], BF16)
        GAINS = sb("GAINS", [128, DEPTH * 26 + 8], F32)
        WMIX = sb("WMIX", [128, DEPTH * 2 * 128], BF16)
        PS = es.enter_context(nc.psum_tensor("PS", [128, 7, T], F32))
        PT = es.enter_context(nc.psum_tensor("PT", [128, 2, T], BF16))

        esem = {e: sem("s_" + e) for e in ("pe", "act", "dve", "pool")}
        wsem = [sem(f"w{b}") for b in range(NB)]
        castsem = [sem(f"cast{l}") for l in range(DEPTH)]
        xsem = sem("xld")
        possem = sem("posld")
        osem = sem("ost")
        csem = sem("cld")
        csem2 = sem("cld2")

        sc = Sched()

        def op(eng, method, *args, r=(), w=(), dma=None, **kw):
            if stopped[0]:
                return None
            return sc.add(eng, lambda e: getattr(e, method)(*args, **kw), r, w, dma)

        bank_ctr = [0]

        def nbank():
            b = bank_ctr[0]
            bank_ctr[0] = (b + 1) % 7
            return b

        ptc = [0]

        def nptb():
            b = ptc[0]
            ptc[0] = 1 - b
            return b

        def PSp(b):
            return ("PS", b)

        def cs(a, n=1):
            return CST[:, a:a + n]

        ident = CB[:, B_ID:B_ID + 128]
        perm = CB[:, B_PERM:B_PERM + 128]
        ones = CB[:, B_ONES:B_ONES + 128]

        def gcol(l, kind, i):
            base = l * 26 + {"mix": 0, "ffn": 8, "mem": 16, "ps": 24}[kind]
            return GAINS[:, base + i:base + i + 1]

        def gfin(i):
            return GAINS[:, DEPTH * 26 + i:DEPTH * 26 + i + 1]

        def ARp(i):
            return ("AR", i)
        QT0, KT0, VT0, SG0, YR0, QX0 = 0, 4, 8, 12, 16, 20

        op("sp", "dma_start", out=CST[:], in_=cst_d[:, :], w=[("CST", 0)], dma=csem)
        last_s = op("sp", "dma_start", out=GAINS[:], in_=gains_d[:, :], w=[("GAINS", 0)], dma=csem)
        sc.mark_written([("CST", 0), ("GAINS", 0)], last_s)
        op("pool", "dma_start", out=CB[:], in_=cstb_d[:, :], w=[("CB", 0)], dma=csem2)
        last_c = op("pool", "dma_start", out=WMIX[:], in_=wmix_d[:, :], w=[("WMIX", 0)], dma=csem2)
        sc.mark_written([("CB", 0), ("WMIX", 0)], last_c)
        for l in range(DEPTH):
            lastop = None
            for name, wd in SLABS:
                off = SLAB_OFF[name][0]
                lastop = op("pool", "dma_start", out=wb_d[l, :, off:off + wd], in_=wf_d[l, :, off:off + wd],
                            dma=castsem[l])
            sc.mark_written([("WB", l)], lastop)
        op("pool", "memset", R32[:], 0.0, w=[("R32", l) for l in range(DEPTH)])
        op("pool", "memset", RBF[:], 0.0, w=[("RBF", l) for l in range(DEPTH)])
        op("pool", "memset", HALO[:], 0.0, w=[("HALO", l) for l in range(DEPTH)])
        op("pool", "memset", MV[:], 0.0, w=[("MV", l) for l in range(DEPTH)])

        stage('loads')
        slab_list = [(l, "memkv") for l in range(DEPTH)]
        for t in range(NT):
            for l in range(DEPTH):
                for name, _ in SLABS[1:]:
                    slab_list.append((l, name))
        ring = {"cur": 0}

        def ring_issue(i):
            if i >= len(slab_list) or stopped[0]:
                return
            l, name = slab_list[i]
            off, wd = SLAB_OFF[name]
            b = i % NB
            op("sp", "dma_start", out=WR[:, b, 0:wd], in_=wb_d[l, :, off:off + wd],
               r=[("WB", l)], w=[("W", b)], dma=wsem[b])

        def ring_next(l, name):
            i = ring["cur"]
            if stopped[0]:
                return i, 0
            assert slab_list[i] == (l, name), (slab_list[i], l, name)
            ring["cur"] = i + 1
            return i, i % NB

        def ring_done(i):
            ring_issue(i + NB)

        for i in range(NB):
            ring_issue(i)

        def rms_stats(src_ap_fn, src_parts, ncols, scale, dst):
            b = nbank()
            for kc in range(KC):
                s = kc % 2
                op("act", "activation", out=SQ[:, s, 0:ncols], in_=src_ap_fn(kc), func=AF.Square,
                   r=[src_parts(kc)], w=[("SQ", s)])
                op("pe", "matmul", PS[:, b, 0:ncols], lhsT=ones, rhs=SQ[:, s, 0:ncols],
                   start=(kc == 0), stop=(kc == KC - 1), r=[("SQ", s), ("CB", 0)], w=[PSp(b)])
            op("act", "activation", out=TT[:, 0:ncols], in_=PS[:, b, 0:ncols], func=AF.Ln,
               scale=scale, bias=cs(C_EPS), r=[PSp(b), ("CST", 0)], w=[("TT", 0)])
            op("act", "activation", out=dst[:, 0:ncols], in_=TT[:, 0:ncols], func=AF.Exp, scale=-0.5,
               r=[("TT", 0)], w=[("RSTD", 0)])

        op("sp", "dma_start", out=X[:, :, 0:MEM], in_=memT_d.rearrange("(kc p) m -> p kc m", p=128),
           w=[("X", kc) for kc in range(KC)], dma=xsem)
        rms_stats(lambda kc: X[:, kc, 0:MEM], lambda kc: ("X", kc), MEM, 1.0 / D, RSTD)
        for l in range(DEPTH):
            for kc in range(KC):
                op("dve", "scalar_tensor_tensor", out=Hh[:, kc, 0:MEM], in0=X[:, kc, 0:MEM],
                   scalar=gcol(l, "mem", kc), in1=RSTD[:, 0:MEM], op0=ALU.mult, op1=ALU.mult,
                   r=[("X", kc), ("RSTD", 0), ("GAINS", 0)], w=[("H", kc)])
            wi, wbuf = ring_next(l, "memkv")
            Wv = WR[:, wbuf, 0:4096].rearrange("p (k n) -> p k n", k=KC)
            for hc in range(2):
                b = nbank()
                for kc in range(KC):
                    op("pe", "matmul", PS[:, b, 0:MEM], lhsT=Wv[:, kc, hc * 128:(hc + 1) * 128],
                       rhs=Hh[:, kc, 0:MEM], start=(kc == 0), stop=(kc == KC - 1),
                       r=[("W", wbuf), ("H", kc)], w=[PSp(b)])
                op("act", "activation", out=MK[:, l, hc, :], in_=PS[:, b, 0:MEM], func=AF.Copy,
                   r=[PSp(b)], w=[("MK", l)])
            for mc in range(2):
                b = nbank()
                for kc in range(KC):
                    op("pe", "matmul", PS[:, b, 0:256], lhsT=Hh[:, kc, mc * 128:(mc + 1) * 128],
                       rhs=Wv[:, kc, 256:512], start=(kc == 0), stop=(kc == KC - 1),
                       r=[("W", wbuf), ("H", kc)], w=[PSp(b)])
                for h in range(H):
                    c0 = (h % 2) * 64
                    op("dve", "tensor_copy", out=MV[:, l, mc, h, c0:c0 + 64], in_=PS[:, b, h * 64:(h + 1) * 64],
                       r=[PSp(b)], w=[("MV", l)])
            ring_done(wi)

        stage('memkv')
        def rope_tables(t):
            op("sp", "dma_start", out=POSI[:], in_=pos_d[0:1, t * T:(t + 1) * T].partition_broadcast(128),
               w=[("POSI", 0)], dma=possem)
            TWO_PI = 6.283185307179586
            C1 = 6.28125
            C2 = TWO_PI - C1
            PI = 3.141592653589793
            PIS = 3.1415925
            op("dve", "tensor_copy", out=RA[:], in_=POSI[:], r=[("POSI", 0)], w=[("RA", 0)])
            op("dve", "tensor_scalar", out=RB[:], in0=RA[:], scalar1=cs(C_INVF), scalar2=None, op0=ALU.mult,
               r=[("RA", 0), ("CST", 0)], w=[("RB", 0)])
            op("dve", "tensor_scalar", out=POSI[:], in0=RB[:], scalar1=1.0 / TWO_PI, scalar2=None, op0=ALU.mult,
               r=[("RB", 0)], w=[("POSI", 0)])
            op("dve", "tensor_copy", out=RA[:], in_=POSI[:], r=[("POSI", 0)], w=[("RA", 0)])
            op("dve", "scalar_tensor_tensor", out=RB[:], in0=RA[:], scalar=-C1, in1=RB[:], op0=ALU.mult, op1=ALU.add,
               r=[("RA", 0), ("RB", 0)], w=[("RB", 0)])
            op("dve", "scalar_tensor_tensor", out=RB[:], in0=RA[:], scalar=-C2, in1=RB[:], op0=ALU.mult, op1=ALU.add,
               r=[("RA", 0), ("RB", 0)], w=[("RB", 0)])
            op("dve", "tensor_scalar", out=RA[:], in0=RB[:], scalar1=-PIS, scalar2=PIS, op0=ALU.max, op1=ALU.min,
               r=[("RB", 0)], w=[("RA", 0)])
            op("act", "activation", out=SIN[:], in_=RA[:], func=AF.Sin, scale=cs(C_SGN),
               r=[("RA", 0), ("CST", 0)], w=[("SIN", 0)])
            op("dve", "tensor_scalar", out=TT[:], in0=RB[:], scalar1=PI / 2, scalar2=None, op0=ALU.add,
               r=[("RB", 0)], w=[("TT", 0)])
            op("dve", "tensor_scalar", out=HR[:], in0=TT[:], scalar1=PI, scalar2=-TWO_PI, op0=ALU.is_gt, op1=ALU.mult,
               r=[("TT", 0)], w=[("HR", 0)])
            op("dve", "tensor_tensor", out=TT[:], in0=TT[:], in1=HR[:], op=ALU.add,
               r=[("TT", 0), ("HR", 0)], w=[("TT", 0)])
            op("dve", "tensor_scalar", out=TT[:], in0=TT[:], scalar1=-PIS, scalar2=PIS, op0=ALU.max, op1=ALU.min,
               r=[("TT", 0)], w=[("TT", 0)])
            op("act", "activation", out=COS[:], in_=TT[:], func=AF.Sin, r=[("TT", 0)], w=[("COS", 0)])

        def norm_to_h(l, kind):
            rms_stats(lambda kc: X[:, kc, :], lambda kc: ("X", kc), T, 1.0 / D, RSTD)
            for kc in range(KC):
                op("dve", "scalar_tensor_tensor", out=Hh[:, kc, :], in0=X[:, kc, :], scalar=gcol(l, kind, kc),
                   in1=RSTD[:], op0=ALU.mult, op1=ALU.mult,
                   r=[("X", kc), ("RSTD", 0), ("GAINS", 0)], w=[("H", kc)])

        def proj_chunk(Wv, wbuf, j, b):
            for kc in range(KC):
                op("pe", "matmul", PS[:, b, :], lhsT=Wv[:, kc, j * 128:(j + 1) * 128], rhs=Hh[:, kc, :],
                   start=(kc == 0), stop=(kc == KC - 1), r=[("W", wbuf), ("H", kc)], w=[PSp(b)])

        import os as _os
        KSUB = int(_os.environ.get("KSUB", "99"))

        def rope_chunk(b, dst_part_idx, qbi):
            if KSUB < 1:
                stopped[0] = True
            op("act", "activation", out=QB[:, qbi, :], in_=PS[:, b, :], func=AF.Copy, r=[PSp(b)], w=[("QB", qbi)])
            if KSUB < 2:
                stopped[0] = True
            b2 = nbank()
            op("pe", "matmul", PS[:, b2, :], lhsT=perm, rhs=QB[:, qbi, :], start=True, stop=True,
               r=[("QB", qbi), ("CB", 0)], w=[PSp(b2)])
            if KSUB < 3:
                stopped[0] = True
            op("dve", "tensor_tensor", out=RA[:], in0=PS[:, b, :], in1=COS[:], op=ALU.mult,
               r=[PSp(b), ("COS", 0)], w=[("RA", 0)])
            op("dve", "tensor_tensor", out=RB[:], in0=PS[:, b2, :], in1=SIN[:], op=ALU.mult,
               r=[PSp(b2), ("SIN", 0)], w=[("RB", 0)])
            if KSUB < 4:
                stopped[0] = True
            op("pool", "tensor_tensor", out=AR[:, dst_part_idx, :], in0=RA[:], in1=RB[:], op=ALU.add,
               r=[("RA", 0), ("RB", 0)], w=[ARp(dst_part_idx)])

        def mixer(t, l):
            norm_to_h(l, "mix")
            stage('norm')
            wi, wbuf = ring_next(l, "q")
            Wv = WR[:, wbuf, 0:4096].rearrange("p (k n) -> p k n", k=KC)
            for h in range(H):
                b = nbank()
                proj_chunk(Wv, wbuf, h, b)
                rope_chunk(b, QT0 + h, h % 2)
            ring_done(wi)
            stage('q')
            wi, wbuf = ring_next(l, "k")
            Wv = WR[:, wbuf, 0:4096].rearrange("p (k n) -> p k n", k=KC)
            for h in range(H):
                b = nbank()
                proj_chunk(Wv, wbuf, h, b)
                rope_chunk(b, KT0 + h, h % 2)
            ring_done(wi)
            stage('k')
            wi, wbuf = ring_next(l, "v")
            Wv = WR[:, wbuf, 0:4096].rearrange("p (k n) -> p k n", k=KC)
            for c in range(NCH):
                b = nbank()
                for kc in range(KC):
                    op("pe", "matmul", PS[:, b, :], lhsT=Hh[:, kc, c * 128:(c + 1) * 128], rhs=Wv[:, kc, :],
                       start=(kc == 0), stop=(kc == KC - 1), r=[("W", wbuf), ("H", kc)], w=[PSp(b)])
                op("act", "activation", out=AR[:, VT0 + c, :], in_=PS[:, b, :], func=AF.Copy,
                   r=[PSp(b)], w=[ARp(VT0 + c)])
            ring_done(wi)
            stage('v')
            wi, wbuf = ring_next(l, "gr")
            Wv = WR[:, wbuf, 0:4096].rearrange("p (k n) -> p k n", k=KC)
            for h in range(H):
                b = nbank()
                proj_chunk(Wv, wbuf, h, b)
                op("act", "activation", out=AR[:, SG0 + h, :], in_=PS[:, b, :], func=AF.Silu,
                   r=[PSp(b)], w=[ARp(SG0 + h)])
            ring_done(wi)
            stage('gr')
            wi, wbuf = ring_next(l, "ux")
            Wv = WR[:, wbuf, 0:4096].rearrange("p (k n) -> p k n", k=KC)
            op("pool", "tensor_copy", out=U[:, :, 0:15], in_=HALO[:, l, :, :], r=[("HALO", l)], w=[("U", 0)])
            for c in range(2):
                b = nbank()
                proj_chunk(Wv, wbuf, c, b)
                op("dve", "tensor_copy", out=U[:, c, 15:15 + T], in_=PS[:, b, :], r=[PSp(b)], w=[("U", 0)])
            for c in range(2):
                b = nbank()
                proj_chunk(Wv, wbuf, 2 + c, b)
                op("act", "activation", out=AR[:, QX0 + c, :], in_=PS[:, b, :], func=AF.Copy,
                   r=[PSp(b)], w=[ARp(QX0 + c)])
            ring_done(wi)

            stage('ux')
            maskT = CST[:, C_MASK:C_MASK + 512]
            ksc = CST[:, C_KSC:C_KSC + 512]
            gct = CST[:, C_GC:C_GC + 512]
            epsr = CST[:, C_EPSR:C_EPSR + 512]
            for c in range(NCH):
                cl = slice(c * 128, (c + 1) * 128)
                bs = nbank()
                for h in range(H):
                    op("pe", "matmul", PS[:, bs, h * 128:(h + 1) * 128], lhsT=AR[:, KT0 + h, cl], rhs=AR[:, QT0 + h, cl],
                       start=True, stop=True, r=[ARp(KT0 + h), ARp(QT0 + h)], w=[PSp(bs)])
                si = c % 2
                op("dve", "tensor_tensor", out=ST[:, si, :], in0=PS[:, bs, :], in1=maskT, op=ALU.mult,
                   r=[PSp(bs), ("CST", 0)], w=[("ST", si)])
                pb = nptb()
                for h in range(H):
                    op("pe", "transpose", PT[:, pb, h * 128:(h + 1) * 128], AR[:, KT0 + h, cl], ident,
                       r=[ARp(KT0 + h), ("CB", 0)], w=[("PT", pb)])
                op("dve", "tensor_tensor", out=KTOK[:, si, :], in0=PT[:, pb, :], in1=ksc, op=ALU.mult,
                   r=[("PT", pb), ("CST", 0)], w=[("KTOK", si)])
                bo = nbank()
                for h in range(H):
                    hs = slice(h * 128, (h + 1) * 128)
                    op("pe", "matmul", PS[:, bo, hs], lhsT=AR[:, VT0 + c, hs], rhs=ST[:, si, hs],
                       start=True, stop=False, r=[ARp(VT0 + c), ("ST", si)], w=[PSp(bo)])
                    op("pe", "matmul", PS[:, bo, hs], lhsT=RBF[:, l, hs], rhs=AR[:, QT0 + h, cl],
                       start=False, stop=True, r=[("RBF", l), ARp(QT0 + h)], w=[PSp(bo)])
                bk = nbank()
                for h in range(H):
                    hs = slice(h * 128, (h + 1) * 128)
                    op("pe", "matmul", PS[:, bk, hs], lhsT=KTOK[:, si, hs], rhs=AR[:, VT0 + c, hs],
                       start=True, stop=True, r=[("KTOK", si), ARp(VT0 + c)], w=[PSp(bk)])
                op("dve", "tensor_tensor", out=Y1[:], in0=R32[:, l, :], in1=PS[:, bk, :], op=ALU.add,
                   r=[("R32", l), PSp(bk)], w=[("Y1", 0)])
                op("pool", "tensor_tensor", out=RBF[:, l, :], in0=Y1[:], in1=gct, op=ALU.mult,
                   r=[("Y1", 0), ("CST", 0)], w=[("RBF", l)])
                op("dve", "tensor_tensor", out=R32[:, l, :], in0=Y1[:], in1=gct, op=ALU.mult,
                   r=[("Y1", 0), ("CST", 0)], w=[("R32", l)])
                op("act", "activation", out=YSQ[:, si, :], in_=PS[:, bo, :], func=AF.Square,
                   r=[PSp(bo)], w=[("YSQ", si)])
                bn = nbank()
                op("pe", "matmul", PS[:, bn, :], lhsT=ones, rhs=YSQ[:, si, :], start=True, stop=True,
                   r=[("YSQ", si), ("CB", 0)], w=[PSp(bn)])
                op("dve", "scalar_tensor_tensor", out=TT[:], in0=PS[:, bn, :], scalar=1.0 / 128, in1=epsr,
                   op0=ALU.mult, op1=ALU.add, r=[PSp(bn), ("CST", 0)], w=[("TT", 0)])
                op("act", "activation", out=TT[:], in_=TT[:], func=AF.Ln, r=[("TT", 0)], w=[("TT", 0)])
                op("act", "activation", out=HR[:], in_=TT[:], func=AF.Exp, scale=-0.5, r=[("TT", 0)], w=[("HR", 0)])
                op("dve", "tensor_tensor", out=RA[:], in0=PS[:, bo, :], in1=HR[:], op=ALU.mult,
                   r=[PSp(bo), ("HR", 0)], w=[("RA", 0)])
                op("pool", "tensor_tensor", out=AR[:, YR0:YR0 + 4, cl],
                   in0=RA[:].rearrange("p (h i) -> p h i", h=H), in1=AR[:, SG0:SG0 + 4, cl], op=ALU.mult,
                   r=[("RA", 0)] + [ARp(SG0 + h) for h in range(H)], w=[ARp(YR0 + h) for h in range(H)])

            stage('ret')
            op("pool", "tensor_tensor", out=PB[:, :, 1:T + 15], in0=U[:, :, 1:T + 15], in1=U[:, :, 0:T + 14], op=ALU.add,
               r=[("U", 0)], w=[("PB", 0)])
            op("pool", "tensor_tensor", out=PC[64:128, 0, 3:T + 15], in0=PB[64:128, 0, 3:T + 15], in1=PB[64:128, 0, 1:T + 13],
               op=ALU.add, r=[("PB", 0)], w=[("PC", 0)])
            op("pool", "tensor_tensor", out=PC[:, 1, 3:T + 15], in0=PB[:, 1, 3:T + 15], in1=PB[:, 1, 1:T + 13],
               op=ALU.add, r=[("PB", 0)], w=[("PC", 0)])
            op("pool", "tensor_tensor", out=PB[:, 1, 7:T + 15], in0=PC[:, 1, 7:T + 15], in1=PC[:, 1, 3:T + 11],
               op=ALU.add, r=[("PC", 0)], w=[("PB", 0)])
            op("pool", "tensor_tensor", out=PC[64:128, 1, 15:T + 15], in0=PB[64:128, 1, 15:T + 15], in1=PB[64:128, 1, 7:T + 7],
               op=ALU.add, r=[("PB", 0)], w=[("PC", 0)])
            wins = [(PB, 0, 0), (PC, 0, 64), (PB, 1, 0), (PC, 1, 64)]
            for g, (buf, c, p0) in enumerate(wins):
                bn_ = "PB" if buf is PB else "PC"
                if t == 0:
                    op("pool", "tensor_tensor", out=buf[p0:p0 + 64, c, 15:31], in0=buf[p0:p0 + 64, c, 15:31],
                       in1=CST[p0:p0 + 64, C_CORR + c * 16:C_CORR + c * 16 + 16], op=ALU.mult,
                       r=[(bn_, 0), ("CST", 0)], w=[(bn_, 0)])
                op("dve", "scalar_tensor_tensor", out=POOLED[p0:p0 + 64, c, :], in0=buf[p0:p0 + 64, c, 15:T + 15],
                   scalar=CST[p0:p0 + 64, C_INVW + c:C_INVW + c + 1], in1=U[p0:p0 + 64, c, 15:T + 15],
                   op0=ALU.mult, op1=ALU.subtract, r=[(bn_, 0), ("U", 0), ("CST", 0)], w=[("POOLED", c)])
            op("pool", "tensor_copy", out=HALO[:, l, :, :], in_=U[:, :, T:T + 15], r=[("U", 0)], w=[("HALO", l)])
            for c in range(2):
                b = nbank()
                op("pe", "matmul", PS[:, b, :], lhsT=WMIX[:, (l * 2 + c) * 128:(l * 2 + c + 1) * 128], rhs=POOLED[:, c, :],
                   start=True, stop=True, r=[("WMIX", 0), ("POOLED", c)], w=[PSp(b)])
                op("act", "activation", out=YPOOL[:, c, :], in_=PS[:, b, :], func=AF.Copy, scale=gcol(l, "ps", c),
                   r=[PSp(b), ("GAINS", 0)], w=[("YPOOL", c)])

            stage('pool')
            for hc in range(2):
                ba = nbank()
                bd = nbank()
                for hh in range(2):
                    h = 2 * hc + hh
                    r0 = hh * 64
                    eb = h % 2
                    bx = [nbank(), nbank()]
                    for mc in range(2):
                        op("pe", "matmul", PS[:, bx[mc], :], lhsT=MK[r0:r0 + 64, l, hc, mc * 128:(mc + 1) * 128],
                           rhs=AR[r0:r0 + 64, QX0 + hc, :], start=True, stop=True,
                           r=[("MK", l), ARp(QX0 + hc)], w=[PSp(bx[mc])])
                    for mc in range(2):
                        op("act", "activation", out=EXPT[:, eb, mc, :], in_=PS[:, bx[mc], :], func=AF.Exp, scale=0.125,
                           r=[PSp(bx[mc])], w=[("EXPT", eb)])
                    for mc in range(2):
                        first = (hh == 0 and mc == 0)
                        last = (hh == 1 and mc == 1)
                        op("pe", "matmul", PS[:, ba, :], lhsT=MV[:, l, mc, h, :], rhs=EXPT[:, eb, mc, :],
                           start=first, stop=last, r=[("MV", l), ("EXPT", eb)], w=[PSp(ba)])
                        op("pe", "matmul", PS[:, bd, :], lhsT=CB[:, B_OPAD + hh * 128:B_OPAD + (hh + 1) * 128],
                           rhs=EXPT[:, eb, mc, :], start=first, stop=last, r=[("CB", 0), ("EXPT", eb)], w=[PSp(bd)])
                op("dve", "reciprocal", out=RDEN[:], in_=PS[:, bd, :], r=[PSp(bd)], w=[("RDEN", 0)])
                op("dve", "tensor_tensor", out=YX[:, hc, :], in0=PS[:, ba, :], in1=RDEN[:], op=ALU.mult,
                   r=[PSp(ba), ("RDEN", 0)], w=[("YX", hc)])

            stage('xattn')
            for c in range(8):
                wi, wbuf = ring_next(l, f"mg{c}")
                Wg = WR[:, wbuf, 0:3072].rearrange("p (k n) -> p k n", k=KC)
                Wr = WR[:, wbuf, 3072:3584].rearrange("p (k n) -> p k n", k=4)
                Wp = WR[:, wbuf, 3584:3840].rearrange("p (k n) -> p k n", k=2)
                Wx = WR[:, wbuf, 3840:4096].rearrange("p (k n) -> p k n", k=2)
                ts = (c % 2) * 3
                bg = []
                for j in range(3):
                    b = nbank()
                    bg.append(b)
                    proj_chunk(Wg, wbuf, j, b)
                    op("act", "activation", out=TG[:, ts + j, :], in_=PS[:, b, :], func=AF.Tanh, scale=0.5,
                       r=[PSp(b)], w=[("TG", ts + j)])
                br = nbank()
                for h in range(H):
                    op("pe", "matmul", PS[:, br, :], lhsT=Wr[:, h, :], rhs=AR[:, YR0 + h, :], start=(h == 0), stop=(h == H - 1),
                       r=[("W", wbuf), ARp(YR0 + h)], w=[PSp(br)])
                bp = nbank()
                for j in range(2):
                    op("pe", "matmul", PS[:, bp, :], lhsT=Wp[:, j, :], rhs=YPOOL[:, j, :], start=(j == 0), stop=(j == 1),
                       r=[("W", wbuf), ("YPOOL", j)], w=[PSp(bp)])
                bxx = nbank()
                for j in range(2):
                    op("pe", "matmul", PS[:, bxx, :], lhsT=Wx[:, j, :], rhs=YX[:, j, :], start=(j == 0), stop=(j == 1),
                       r=[("W", wbuf), ("YX", j)], w=[PSp(bxx)])
                ring_done(wi)
                for j, bb in enumerate((br, bp, bxx)):
                    op("dve", "scalar_tensor_tensor", out=Mm[:, ts + j, :], in0=TG[:, ts + j, :], scalar=1.0, in1=PS[:, bb, :],
                       op0=ALU.add, op1=ALU.mult, r=[("TG", ts + j), PSp(bb)], w=[("Mm", ts + j)])
                op("pool", "tensor_tensor", out=Mm[:, ts, :], in0=Mm[:, ts, :], in1=Mm[:, ts + 1, :], op=ALU.add,
                   r=[("Mm", ts), ("Mm", ts + 1)], w=[("Mm", ts)])
                op("pool", "tensor_tensor", out=AR[:, c, :], in0=Mm[:, ts, :], in1=Mm[:, ts + 2, :], op=ALU.add,
                   r=[("Mm", ts), ("Mm", ts + 2)], w=[ARp(c)])

            stage('merge')
            for s in range(2):
                wi, wbuf = ring_next(l, f"wo{s}")
                Wv = WR[:, wbuf, 0:4096].rearrange("p (k n) -> p k n", k=KC)
                for j in range(4):
                    oc = s * 4 + j
                    b = nbank()
                    for kc in range(KC):
                        op("pe", "matmul", PS[:, b, :], lhsT=Wv[:, kc, j * 128:(j + 1) * 128], rhs=AR[:, kc, :],
                           start=(kc == 0), stop=(kc == KC - 1), r=[("W", wbuf), ARp(kc)], w=[PSp(b)])
                    op("dve", "scalar_tensor_tensor", out=X[:, oc, :], in0=PS[:, b, :], scalar=0.5, in1=X[:, oc, :],
                       op0=ALU.mult, op1=ALU.add, r=[PSp(b), ("X", oc)], w=[("X", oc)])
                ring_done(wi)

        def ffn(t, l):
            norm_to_h(l, "ffn")
            for s in range(11):
                wi, wbuf = ring_next(l, f"fi{s}")
                Wv = WR[:, wbuf, 0:4096].rearrange("p (k n) -> p k n", k=KC)
                for j in range(2):
                    c = 2 * s + j
                    ba = nbank()
                    proj_chunk(Wv, wbuf, j, ba)
                    bb = nbank()
                    proj_chunk(Wv, wbuf, 2 + j, bb)
                    ti = c % 2
                    op("act", "activation", out=TH[:, ti, :], in_=PS[:, ba, :], func=AF.Silu,
                       r=[PSp(ba)], w=[("TH", ti)])
                    op("dve", "tensor_tensor", out=AR[:, c, :], in0=TH[:, ti, :], in1=PS[:, bb, :], op=ALU.mult,
                       r=[("TH", ti), PSp(bb)], w=[ARp(c)])
                ring_done(wi)
            for oc in range(8):
                wi, wbuf = ring_next(l, f"fo{oc}")
                Wv = WR[:, wbuf, 0:2816].rearrange("p (k n) -> p k n", k=FKC)
                b = nbank()
                for kc in range(FKC):
                    op("pe", "matmul", PS[:, b, :], lhsT=Wv[:, kc, :], rhs=AR[:, kc, :],
                       start=(kc == 0), stop=(kc == FKC - 1), r=[("W", wbuf), ARp(kc)], w=[PSp(b)])
                op("dve", "tensor_tensor", out=X[:, oc, :], in0=PS[:, b, :], in1=X[:, oc, :], op=ALU.add,
                   r=[PSp(b), ("X", oc)], w=[("X", oc)])
                ring_done(wi)

        xT_v = xT_d.rearrange("(kc p) s -> p kc s", p=128)
        out_v = out_d.rearrange("(kc p) s -> p kc s", p=128)
        last_store = None
        for t in range(NT):
            op("sp", "dma_start", out=X[:], in_=xT_v[:, :, t * T:(t + 1) * T],
               w=[("X", kc) for kc in range(KC)], dma=xsem)
            rope_tables(t)
            for l in range(DEPTH):
                mixer(t, l)
                ffn(t, l)
            rms_stats(lambda kc: X[:, kc, :], lambda kc: ("X", kc), T, 1.0 / D, RSTD)
            for kc in range(KC):
                op("dve", "scalar_tensor_tensor", out=X[:, kc, :], in0=X[:, kc, :], scalar=gfin(kc),
                   in1=RSTD[:], op0=ALU.mult, op1=ALU.mult,
                   r=[("X", kc), ("RSTD", 0), ("GAINS", 0)], w=[("X", kc)])
            last_store = op("sp", "dma_start", out=out_v[:, :, t * T:(t + 1) * T], in_=X[:],
                            r=[("X", kc) for kc in range(KC)], dma=osem)
        if stopped[0]:
            stopped[0] = False
            last_store = op("sp", "dma_start", out=out_v[:, :, 0:T], in_=X[:],
                            r=[("X", kc) for kc in range(KC)] + [("RSTD", 0)], dma=osem)
        else:
            assert ring["cur"] == len(slab_list)

        block = es.enter_context(nc.Block())

        @block.sync
        def _(e):
            sc.emit("sp", e, esem)
            e.wait_ge(osem, last_store.val)

        @block.tensor
        def _(e):
            sc.emit("pe", e, esem)

        @block.scalar
        def _(e):
            sc.emit("act", e, esem)

        @block.vector
        def _(e):
            sc.emit("dve", e, esem)

        @block.gpsimd
        def _(e):
            sc.emit("pool", e, esem)

    return nc


def _slab(Wm, cols):
    K = Wm.shape[0]
    sub = Wm[:, cols]
    return np.ascontiguousarray(sub.reshape(K // 128, 128, -1).transpose(1, 0, 2)).reshape(128, -1)


def _layer_line(l, w_in, w_up_ret, w_up_pool, w_up_x, w_out, w_ffn_in, w_ffn_out, w_mem_kv):
    ar = np.arange
    parts = {}
    parts["memkv"] = _slab(w_mem_kv[l], ar(512))
    parts["q"] = _slab(w_in[l], ar(0, 512))
    parts["k"] = _slab(w_in[l], ar(512, 1024))
    parts["v"] = _slab(w_in[l], ar(1024, 1536))
    parts["gr"] = _slab(w_in[l], ar(1536, 2048))
    parts["ux"] = _slab(w_in[l], ar(2048, 2560))
    for c in range(8):
        gcols = np.concatenate([2560 + j * 1024 + c * 128 + ar(128) for j in range(3)])
        oc = c * 128 + ar(128)
        parts[f"mg{c}"] = np.concatenate(
            [_slab(w_in[l], gcols), _slab(w_up_ret[l], oc), _slab(w_up_pool[l], oc), _slab(w_up_x[l], oc)], axis=1)
    for s in range(2):
        parts[f"wo{s}"] = _slab(w_out[l], ar(s * 512, (s + 1) * 512))
    for s in range(11):
        cols = np.concatenate([(2 * s) * 128 + ar(128), (2 * s + 1) * 128 + ar(128),
                               FH + (2 * s) * 128 + ar(128), FH + (2 * s + 1) * 128 + ar(128)])
        parts[f"fi{s}"] = _slab(w_ffn_in[l], cols)
    for j in range(8):
        parts[f"fo{j}"] = _slab(w_ffn_out[l], ar(j * 128, (j + 1) * 128))
    line = np.concatenate([parts[n] for n, _ in SLABS], axis=1)
    assert line.shape == (128, LINE), line.shape
    return line


def _consts():
    c = np.zeros((128, NCONST), np.float64)
    g = 1.0 - np.exp2(-5.0 - np.arange(H))
    j = np.arange(128)
    dk = 128.0 ** -0.5
    for h in range(H):
        ginv = g[h] ** (-(j + 1.0))
        m = (j[:, None] <= j[None, :]) * (ginv[:, None] * dk)
        c[:, C_MASK + h * 128:C_MASK + (h + 1) * 128] = m
        c[:, C_KSC + h * 128:C_KSC + (h + 1) * 128] = (ginv * dk)[:, None]
        c[:, C_GC + h * 128:C_GC + (h + 1) * 128] = g[h] ** 128.0
        c[:, C_EPSR + h * 128:C_EPSR + (h + 1) * 128] = (EPS * g[h] ** (-2.0 * (j + 1.0)))[None, :]
    wins = (2, 4, 8, 16)
    for ch in range(2):
        for half in range(2):
            w = wins[ch * 2 + half]
            p = slice(half * 64, half * 64 + 64)
            c[p, C_INVW + ch] = 1.0 / w
            tt = np.arange(16)
            c[p, C_CORR + ch * 16:C_CORR + ch * 16 + 16] = (w / np.minimum(tt + 1, w))[None, :]
    inv_freq = (10000.0 ** (-np.arange(0, 128, 2, dtype=np.float32) / np.float32(128))).astype(np.float32)
    c[:, C_INVF] = np.concatenate([inv_freq, inv_freq])
    c[0:64, C_SGN] = -1.0
    c[64:128, C_SGN] = 1.0
    c[:, C_EPS] = EPS
    cb = np.zeros((128, NCB), np.float32)
    cb[:, B_ID:B_ID + 128] = np.eye(128)
    pm = np.zeros((128, 128), np.float32)
    mm = np.arange(128)
    pm[(mm + 64) % 128, mm] = 1.0
    cb[:, B_PERM:B_PERM + 128] = pm
    cb[:, B_ONES:B_ONES + 128] = 1.0
    cb[:, B_OPAD + 0:B_OPAD + 64] = 1.0
    cb[:, B_OPAD + 128 + 64:B_OPAD + 256] = 1.0
    return c.astype(np.float32), cb


_PROG_CACHE = {}


def kernel(x, mem, positions, norm_mix, w_in, w_up_ret, w_pool_mix, pool_scale, w_up_pool,
           norm_mem, w_mem_kv, w_up_x, w_out, norm_ffn, w_ffn_in, w_ffn_out, final_norm):
    x = np.asarray(x, np.float32)
    mem = np.asarray(mem, np.float32)
    B, S, _ = x.shape
    DEPTH = int(np.asarray(norm_mix).shape[0])
    f = lambda a: np.asarray(a, np.float32)
    w_in, w_up_ret, w_up_pool, w_up_x, w_out = f(w_in), f(w_up_ret), f(w_up_pool), f(w_up_x), f(w_out)
    w_ffn_in, w_ffn_out, w_mem_kv, w_pool_mix = f(w_ffn_in), f(w_ffn_out), f(w_mem_kv), f(w_pool_mix)
    norm_mix, norm_ffn, norm_mem, pool_scale, final_norm = f(norm_mix), f(norm_ffn), f(norm_mem), f(pool_scale), f(final_norm)

    wf = np.stack([_layer_line(l, w_in, w_up_ret, w_up_pool, w_up_x, w_out, w_ffn_in, w_ffn_out, w_mem_kv)
                   for l in range(DEPTH)], axis=0)
    cst, cstb = _consts()
    gains = np.zeros((128, DEPTH * 26 + 8), np.float32)
    for l in range(DEPTH):
        gains[:, l * 26 + 0:l * 26 + 8] = norm_mix[l].reshape(8, 128).T
        gains[:, l * 26 + 8:l * 26 + 16] = norm_ffn[l].reshape(8, 128).T
        gains[:, l * 26 + 16:l * 26 + 24] = norm_mem[l].reshape(8, 128).T
        gains[:, l * 26 + 24:l * 26 + 26] = pool_scale[l].reshape(2, 128).T
    gains[:, DEPTH * 26:DEPTH * 26 + 8] = final_norm.reshape(8, 128).T
    wmix = np.zeros((128, DEPTH, 2, 128), np.float32)
    for l in range(DEPTH):
        for g in range(4):
            c, half = g // 2, g % 2
            wmix[half * 64:half * 64 + 64, l, c, half * 64:half * 64 + 64] = w_pool_mix[l, g]
    wmix = wmix.reshape(128, -1)
    pos = np.asarray(positions, np.int32).reshape(1, S)

    import os
    stop = int(os.environ.get("KSTOP", "0")) or None
    key = (S, DEPTH)
    if key not in _PROG_CACHE:
        _PROG_CACHE[key] = build_program(S, DEPTH, stop)
    nc = _PROG_CACHE[key]
    in_maps = []
    for b in range(B):
        in_maps.append({
            "xT": np.ascontiguousarray(x[b].T), "memT": np.ascontiguousarray(mem[b].T), "pos": pos,
            "wf": wf, "cst": cst, "cstb": cstb, "gains": gains, "wmix": wmix,
        })
    res = run_bass_kernel_spmd(nc, in_maps, core_ids=list(range(B)))
    out = np.stack([np.ascontiguousarray(res.results[b]["outT"].T) for b in range(B)], axis=0)
    return out.astype(np.float32)
```

```python
import contextlib
import numpy as np
import concourse.bass as bass
import concourse.mybir as mybir
from concourse.bass_utils import run_bass_kernel_spmd

F32, BF16, I32 = mybir.dt.float32, mybir.dt.bfloat16, mybir.dt.int32
ALU = mybir.AluOpType
AF = mybir.ActivationFunctionType

D = 1024
KC = 8
T = 512
NCH = T // 128
H = 4
MEM = 256
FH = 2816
FKC = 22
IN_W = 5632
EPS = 1e-6
NB = 5
SLABW = 4096

SLABS = [("memkv", 4096), ("q", 4096), ("k", 4096), ("v", 4096), ("gr", 4096), ("ux", 4096)]
SLABS += [(f"mg{c}", 4096) for c in range(8)]
SLABS += [("wo0", 4096), ("wo1", 4096)]
SLABS += [(f"fi{s}", 4096) for s in range(11)]
SLABS += [(f"fo{j}", 2816) for j in range(8)]
SLAB_OFF = {}
_o = 0
for _n, _w in SLABS:
    SLAB_OFF[_n] = (_o, _w)
    _o += _w
LINE = _o

C_MASK = 0
C_KSC = 512
C_GC = 1024
C_EPSR = 1536
C_INVW = 2048
C_CORR = 2050
C_INVF = 2114
C_SGN = 2115
C_EPS = 2116
C_SG = 2117
C_KEEP = 2118
NCONST = 2120
B_ID = 0
B_PERM = 128
B_ONES = 256
B_OPAD = 384
NCB = 640


class Op:
    __slots__ = ("eng", "fn", "waits", "idx", "needed", "dma", "val", "inc")


class Sched:
    ENGS = ("pe", "act", "dve", "pool", "sp")

    def __init__(self):
        self.streams = {e: [] for e in self.ENGS}
        self.last_w = {}
        self.readers = {}
        self.known = {e: {} for e in self.ENGS}
        self.dma_count = {}

    def add(self, eng, fn, reads=(), writes=(), dma=None, inc=16):
        op = Op()
        op.eng, op.fn, op.needed, op.dma, op.val = eng, fn, False, dma, None
        op.inc = inc
        st = self.streams[eng]
        op.idx = len(st)
        deps = []
        for p in reads:
            w = self.last_w.get(p)
            if w is not None:
                deps.append((w, True))
            if p[0] in ("PS", "PT"):
                rd = self.readers.get(p)
                if rd:
                    for k_, r in rd.items():
                        if k_ != eng:
                            deps.append((r, True))
        for p in writes:
            w = self.last_w.get(p)
            if w is not None:
                deps.append((w, True))
            rd = self.readers.get(p)
            if rd:
                for r in rd.values():
                    deps.append((r, False))
        waits = {}
        kn = self.known[eng]
        for d, hazard_rw in deps:
            if d is op:
                continue
            if d.dma is not None:
                key = ("dma", id(d.dma))
                if kn.get(key, 0) >= d.val:
                    continue
                kn[key] = d.val
                waits[key] = d
                continue
            if d.eng == eng:
                if eng == "pe" or eng == "sp":
                    continue
            key = d.eng
            if kn.get(key, -1) >= d.idx:
                continue
            kn[key] = d.idx
            d.needed = True
            prev = waits.get(key)
            if prev is None or prev.idx < d.idx:
                waits[key] = d
        op.waits = list(waits.values())
        if dma is not None:
            c = self.dma_count.get(id(dma), 0) + 1
            self.dma_count[id(dma)] = c
            op.val = inc * c
        st.append(op)
        for p in reads:
            self.readers.setdefault(p, {})[eng if dma is None else ("dma", id(op))] = op
        for p in writes:
            self.last_w[p] = op
            self.readers[p] = {}
        return op

    def mark_written(self, parts, op):
        for p in parts:
            self.last_w[p] = op
            self.readers[p] = {}

    def emit(self, eng, e, esem):
        if not hasattr(self, "_vals"):
            self._vals = True
            for en in self.ENGS:
                c = 0
                for op in self.streams[en]:
                    if op.dma is None and op.needed:
                        c += 1
                        op.val = c
        for op in self.streams[eng]:
            for d in op.waits:
                if d.dma is not None:
                    e.wait_ge(d.dma, d.val)
                else:
                    e.wait_ge(esem[d.eng], d.val)
            ins = op.fn(e)
            if op.dma is not None:
                ins.then_inc(op.dma, op.inc)
            elif op.needed:
                ins.then_inc(esem[eng], 1)


class _Stop(Exception):
    pass


def build_program(S, DEPTH, npair=4, stop=None):
    NT = S // T + 1
    SP_ = NT * T
    stage_ctr = [0]
    stopped = [False]

    def stage(name):
        stage_ctr[0] += 1
        if stop is not None and stage_ctr[0] >= stop:
            if not stopped[0]:
                print('STOP at stage', stage_ctr[0], name)
            stopped[0] = True
    nc = bass.Bass("TRN2", target_bir_lowering=False)
    xT_d = nc.dram_tensor("xT", [D, SP_], F32, kind="ExternalInput").ap()
    memT_d = nc.dram_tensor("memT", [D, MEM], F32, kind="ExternalInput").ap()
    pos_d = nc.dram_tensor("pos", [1, SP_], I32, kind="ExternalInput").ap()
    wf_d = nc.dram_tensor("wf", [DEPTH, 128, LINE], F32, kind="ExternalInput").ap()
    cst_d = nc.dram_tensor("cst", [128, NCONST], F32, kind="ExternalInput").ap()
    cstb_d = nc.dram_tensor("cstb", [128, NCB], F32, kind="ExternalInput").ap()
    gains_d = nc.dram_tensor("gains", [128, DEPTH * 26 + 8], F32, kind="ExternalInput").ap()
    wmix_d = nc.dram_tensor("wmix", [128, DEPTH * 2 * 128], F32, kind="ExternalInput").ap()
    out_d = nc.dram_tensor("outT", [D, SP_], F32, kind="ExternalOutput").ap()
    snd_d = nc.dram_tensor("snd", [128, KC * T], F32, kind="Internal").ap()
    rcv_d = nc.dram_tensor("rcv", [256, KC * T], F32, kind="Internal", addr_space="Local").ap()
    wb_d = nc.dram_tensor("wb", [DEPTH, 128, LINE], BF16, kind="Internal").ap()

    es = contextlib.ExitStack()
    with es:
        def sb(name, shape, dt):
            return es.enter_context(nc.sbuf_tensor(name, shape, dt))

        def sem(name):
            return es.enter_context(nc.semaphore(name))

        X = sb("X", [128, KC, T], F32)
        Hh = sb("Hh", [128, KC, T], BF16)
        SQ = sb("SQ", [128, 2, T], BF16)
        RSTD = sb("RSTD", [128, T], F32)
        AR = sb("AR", [128, 22, T], BF16)
        RA = sb("RA", [128, T], F32)
        RB = sb("RB", [128, T], F32)
        QB = sb("QB", [128, 2, T], BF16)
        U = sb("U", [128, 2, T + 15], F32)
        PB = sb("PB", [128, 2, T + 15], F32)
        PC = sb("PC", [128, 2, T + 15], F32)
        POOLED = sb("POOLED", [128, 2, T], BF16)
        YPOOL = sb("YPOOL", [128, 2, T], BF16)
        EXPT = sb("EXPT", [128, 2, 2, T], BF16)
        YX = sb("YX", [128, 2, T], BF16)
        RDEN = sb("RDEN", [128, T], F32)
        ST = sb("ST", [128, 2, T], BF16)
        KTOK = sb("KTOK", [128, 2, T], BF16)
        YSQ = sb("YSQ", [128, 2, T], BF16)
        TT = sb("TT", [128, T], F32)
        HR = sb("HR", [128, T], F32)
        Y1 = sb("Y1", [128, T], F32)
        TG = sb("TG", [128, 6, T], F32)
        Mm = sb("Mm", [128, 6, T], F32)
        TH = sb("TH", [128, 2, T], F32)
        R32 = sb("R32", [128, DEPTH, T], F32)
        RBF = sb("RBF", [128, DEPTH, T], BF16)
        HALO = sb("HALO", [128, DEPTH, 2, 15], F32)
        MK = sb("MK", [128, DEPTH, 2, MEM], BF16)
        MV = sb("MV", [128, DEPTH, 2, 4, 128], BF16)
        WR = sb("WR", [128, NB, SLABW], BF16)
        COS = sb("COS", [128, T], F32)
        SIN = sb("SIN", [128, T], F32)
        POSI = sb("POSI", [128, T], I32)
        STG = sb("STG", [128, 2, T], F32)
        CST = sb("CST", [128, NCONST], F32)
        CB = sb("CB", [128, NCB], BF16)
        GAINS = sb("GAINS", [128, DEPTH * 26 + 8], F32)
        WMIX = sb("WMIX", [128, DEPTH * 2 * 128], BF16)
        PS = es.enter_context(nc.psum_tensor("PS", [128, 7, T], F32))
        PT = es.enter_context(nc.psum_tensor("PT", [128, 2, T], BF16))

        esem = {e: sem("s_" + e) for e in ("pe", "act", "dve", "pool")}
        wsem = [sem(f"w{b}") for b in range(NB)]
        castsem = [sem(f"cast{l}") for l in range(DEPTH)]
        xsem = sem("xld")
        possem = sem("posld")
        osem = sem("ost")
        csem = sem("cld")
        csem2 = sem("cld2")
        stgsem = [sem("stg0"), sem("stg1")]
        sndsem = sem("snd")
        ccsem = sem("cc")

        sc = Sched()

        def op(eng, method, *args, r=(), w=(), dma=None, inc=16, **kw):
            if stopped[0]:
                return None
            return sc.add(eng, lambda e: getattr(e, method)(*args, **kw), r, w, dma, inc)

        bank_ctr = [0]

        reserved = set()

        def nbank():
            while True:
                b = bank_ctr[0]
                bank_ctr[0] = (b + 1) % 7
                if b not in reserved:
                    return b

        ptc = [0]

        def nptb():
            b = ptc[0]
            ptc[0] = 1 - b
            return b

        def PSp(b):
            return ("PS", b)

        def cs(a, n=1):
            return CST[:, a:a + n]

        ident = CB[:, B_ID:B_ID + 128]
        perm = CB[:, B_PERM:B_PERM + 128]
        ones = CB[:, B_ONES:B_ONES + 128]

        def gcol(l, kind, i):
            base = l * 26 + {"mix": 0, "ffn": 8, "mem": 16, "ps": 24}[kind]
            return GAINS[:, base + i:base + i + 1]

        def gfin(i):
            return GAINS[:, DEPTH * 26 + i:DEPTH * 26 + i + 1]

        def ARp(i):
            return ("AR", i)
        QT0, KT0, VT0, SG0, YR0, QX0 = 0, 4, 8, 12, 16, 20

        op("sp", "dma_start", out=CST[:], in_=cst_d[:, :], w=[("CST", 0)], dma=csem)
        last_s = op("sp", "dma_start", out=GAINS[:], in_=gains_d[:, :], w=[("GAINS", 0)], dma=csem)
        sc.mark_written([("CST", 0), ("GAINS", 0)], last_s)
        op("pool", "dma_start", out=CB[:], in_=cstb_d[:, :], w=[("CB", 0)], dma=csem2)
        last_c = op("pool", "dma_start", out=WMIX[:], in_=wmix_d[:, :], w=[("WMIX", 0)], dma=csem2)
        sc.mark_written([("CB", 0), ("WMIX", 0)], last_c)
        for l in range(DEPTH):
            lastop = None
            for name, wd in SLABS:
                off = SLAB_OFF[name][0]
                lastop = op("pool", "dma_start", out=wb_d[l, :, off:off + wd], in_=wf_d[l, :, off:off + wd],
                            dma=castsem[l])
            sc.mark_written([("WB", l)], lastop)
        op("pool", "memset", R32[:], 0.0, w=[("R32", l) for l in range(DEPTH)])
        op("pool", "memset", RBF[:], 0.0, w=[("RBF", l) for l in range(DEPTH)])
        op("pool", "memset", HALO[:], 0.0, w=[("HALO", l) for l in range(DEPTH)])
        op("pool", "memset", MV[:], 0.0, w=[("MV", l) for l in range(DEPTH)])

        stage('loads')
        slab_list = [(l, "memkv") for l in range(DEPTH)]
        for t in range(NT):
            for l in range(DEPTH):
                for name, _ in SLABS[1:]:
                    slab_list.append((l, name))
        ring = {"cur": 0}

        def ring_issue(i):
            if i >= len(slab_list) or stopped[0]:
                return
            l, name = slab_list[i]
            off, wd = SLAB_OFF[name]
            b = i % NB
            op("sp", "dma_start", out=WR[:, b, 0:wd], in_=wb_d[l, :, off:off + wd],
               r=[("WB", l)], w=[("W", b)], dma=wsem[b])

        def ring_next(l, name):
            i = ring["cur"]
            if stopped[0]:
                return i, 0
            assert slab_list[i] == (l, name), (slab_list[i], l, name)
            ring["cur"] = i + 1
            return i, i % NB

        def ring_done(i):
            ring_issue(i + NB)

        for i in range(NB):
            ring_issue(i)

        def rms_stats(src_ap_fn, src_parts, ncols, scale, dst):
            b = nbank()
            for kc in range(KC):
                s = kc % 2
                op("act", "activation", out=SQ[:, s, 0:ncols], in_=src_ap_fn(kc), func=AF.Square,
                   r=[src_parts(kc)], w=[("SQ", s)])
                op("pe", "matmul", PS[:, b, 0:ncols], lhsT=ones, rhs=SQ[:, s, 0:ncols],
                   start=(kc == 0), stop=(kc == KC - 1), r=[("SQ", s), ("CB", 0)], w=[PSp(b)])
            op("act", "activation", out=TT[:, 0:ncols], in_=PS[:, b, 0:ncols], func=AF.Ln,
               scale=scale, bias=cs(C_EPS), r=[PSp(b), ("CST", 0)], w=[("TT", 0)])
            op("act", "activation", out=dst[:, 0:ncols], in_=TT[:, 0:ncols], func=AF.Exp, scale=-0.5,
               r=[("TT", 0)], w=[("RSTD", 0)])

        op("sp", "dma_start", out=X[:, :, 0:MEM], in_=memT_d.rearrange("(kc p) m -> p kc m", p=128),
           w=[("X", kc) for kc in range(KC)], dma=xsem)
        rms_stats(lambda kc: X[:, kc, 0:MEM], lambda kc: ("X", kc), MEM, 1.0 / D, RSTD)
        for l in range(DEPTH):
            for kc in range(KC):
                op("dve", "scalar_tensor_tensor", out=Hh[:, kc, 0:MEM], in0=X[:, kc, 0:MEM],
                   scalar=gcol(l, "mem", kc), in1=RSTD[:, 0:MEM], op0=ALU.mult, op1=ALU.mult,
                   r=[("X", kc), ("RSTD", 0), ("GAINS", 0)], w=[("H", kc)])
            wi, wbuf = ring_next(l, "memkv")
            Wv = WR[:, wbuf, 0:4096].rearrange("p (k n) -> p k n", k=KC)
            for hc in range(2):
                b = nbank()
                for kc in range(KC):
                    op("pe", "matmul", PS[:, b, 0:MEM], lhsT=Wv[:, kc, hc * 128:(hc + 1) * 128],
                       rhs=Hh[:, kc, 0:MEM], start=(kc == 0), stop=(kc == KC - 1),
                       r=[("W", wbuf), ("H", kc)], w=[PSp(b)])
                op("act", "activation", out=MK[:, l, hc, :], in_=PS[:, b, 0:MEM], func=AF.Copy,
                   r=[PSp(b)], w=[("MK", l)])
            for mc in range(2):
                b = nbank()
                for kc in range(KC):
                    op("pe", "matmul", PS[:, b, 0:256], lhsT=Hh[:, kc, mc * 128:(mc + 1) * 128],
                       rhs=Wv[:, kc, 256:512], start=(kc == 0), stop=(kc == KC - 1),
                       r=[("W", wbuf), ("H", kc)], w=[PSp(b)])
                for h in range(H):
                    c0 = (h % 2) * 64
                    op("dve", "tensor_copy", out=MV[:, l, mc, h, c0:c0 + 64], in_=PS[:, b, h * 64:(h + 1) * 64],
                       r=[PSp(b)], w=[("MV", l)])
            ring_done(wi)

        stage('memkv')
        def rope_tables(t):
            op("sp", "dma_start", out=POSI[:], in_=pos_d[0:1, t * T:(t + 1) * T].partition_broadcast(128),
               w=[("POSI", 0)], dma=possem)
            TWO_PI = 6.283185307179586
            C1 = 6.28125
            C2 = TWO_PI - C1
            PI = 3.141592653589793
            PIS = 3.1415925
            op("dve", "tensor_copy", out=RA[:], in_=POSI[:], r=[("POSI", 0)], w=[("RA", 0)])
            op("dve", "tensor_scalar", out=RB[:], in0=RA[:], scalar1=cs(C_INVF), scalar2=None, op0=ALU.mult,
               r=[("RA", 0), ("CST", 0)], w=[("RB", 0)])
            op("dve", "tensor_scalar", out=POSI[:], in0=RB[:], scalar1=1.0 / TWO_PI, scalar2=None, op0=ALU.mult,
               r=[("RB", 0)], w=[("POSI", 0)])
            op("dve", "tensor_copy", out=RA[:], in_=POSI[:], r=[("POSI", 0)], w=[("RA", 0)])
            op("dve", "scalar_tensor_tensor", out=RB[:], in0=RA[:], scalar=-C1, in1=RB[:], op0=ALU.mult, op1=ALU.add,
               r=[("RA", 0), ("RB", 0)], w=[("RB", 0)])
            op("dve", "scalar_tensor_tensor", out=RB[:], in0=RA[:], scalar=-C2, in1=RB[:], op0=ALU.mult, op1=ALU.add,
               r=[("RA", 0), ("RB", 0)], w=[("RB", 0)])
            op("dve", "tensor_scalar", out=RA[:], in0=RB[:], scalar1=-PIS, scalar2=PIS, op0=ALU.max, op1=ALU.min,
               r=[("RB", 0)], w=[("RA", 0)])
            op("act", "activation", out=SIN[:], in_=RA[:], func=AF.Sin, scale=cs(C_SGN),
               r=[("RA", 0), ("CST", 0)], w=[("SIN", 0)])
            op("dve", "tensor_scalar", out=TT[:], in0=RB[:], scalar1=PI / 2, scalar2=None, op0=ALU.add,
               r=[("RB", 0)], w=[("TT", 0)])
            op("dve", "tensor_scalar", out=HR[:], in0=TT[:], scalar1=PI, scalar2=-TWO_PI, op0=ALU.is_gt, op1=ALU.mult,
               r=[("TT", 0)], w=[("HR", 0)])
            op("dve", "tensor_tensor", out=TT[:], in0=TT[:], in1=HR[:], op=ALU.add,
               r=[("TT", 0), ("HR", 0)], w=[("TT", 0)])
            op("dve", "tensor_scalar", out=TT[:], in0=TT[:], scalar1=-PIS, scalar2=PIS, op0=ALU.max, op1=ALU.min,
               r=[("TT", 0)], w=[("TT", 0)])
            op("act", "activation", out=COS[:], in_=TT[:], func=AF.Sin, r=[("TT", 0)], w=[("COS", 0)])

        stats = {"bank": None}

        def stats_acc(kc):
            if kc == 0:
                b = nbank()
                reserved.add(b)
                stats["bank"] = b
            b = stats["bank"]
            sq = kc % 2
            op("act", "activation", out=SQ[:, sq, :], in_=X[:, kc, :], func=AF.Square,
               r=[("X", kc)], w=[("SQ", sq)])
            op("pe", "matmul", PS[:, b, :], lhsT=ones, rhs=SQ[:, sq, :], start=(kc == 0), stop=(kc == KC - 1),
               r=[("SQ", sq), ("CB", 0)], w=[PSp(b)])

        def stats_finish():
            b = stats["bank"]
            assert b is not None
            op("act", "activation", out=TT[:], in_=PS[:, b, :], func=AF.Ln, scale=1.0 / D, bias=cs(C_EPS),
               r=[PSp(b), ("CST", 0)], w=[("TT", 0)])
            op("act", "activation", out=RSTD[:], in_=TT[:], func=AF.Exp, scale=-0.5, r=[("TT", 0)], w=[("RSTD", 0)])
            reserved.discard(b)
            stats["bank"] = None

        def norm_to_h(l, kind):
            stats_finish()
            for kc in range(KC):
                op("dve", "scalar_tensor_tensor", out=Hh[:, kc, :], in0=X[:, kc, :], scalar=gcol(l, kind, kc),
                   in1=RSTD[:], op0=ALU.mult, op1=ALU.mult,
                   r=[("X", kc), ("RSTD", 0), ("GAINS", 0)], w=[("H", kc)])

        def proj_chunk(Wv, wbuf, j, b):
            for kc in range(KC):
                op("pe", "matmul", PS[:, b, :], lhsT=Wv[:, kc, j * 128:(j + 1) * 128], rhs=Hh[:, kc, :],
                   start=(kc == 0), stop=(kc == KC - 1), r=[("W", wbuf), ("H", kc)], w=[PSp(b)])

        def rope_chunk(b, dst_part_idx, qbi):
            op("act", "activation", out=QB[:, qbi, :], in_=PS[:, b, :], func=AF.Copy, r=[PSp(b)], w=[("QB", qbi)])
            b2 = nbank()
            op("pe", "matmul", PS[:, b2, :], lhsT=perm, rhs=QB[:, qbi, :], start=True, stop=True,
               r=[("QB", qbi), ("CB", 0)], w=[PSp(b2)])
            op("dve", "tensor_tensor", out=RA[:], in0=PS[:, b, :], in1=COS[:], op=ALU.mult,
               r=[PSp(b), ("COS", 0)], w=[("RA", 0)])
            op("dve", "tensor_tensor", out=RB[:], in0=PS[:, b2, :], in1=SIN[:], op=ALU.mult,
               r=[PSp(b2), ("SIN", 0)], w=[("RB", 0)])
            op("pool", "tensor_tensor", out=AR[:, dst_part_idx, :], in0=RA[:], in1=RB[:], op=ALU.add,
               r=[("RA", 0), ("RB", 0)], w=[ARp(dst_part_idx)])

        def mixer(t, l):
            norm_to_h(l, "mix")
            stage('norm')
            wi, wbuf = ring_next(l, "q")
            Wv = WR[:, wbuf, 0:4096].rearrange("p (k n) -> p k n", k=KC)
            for h in range(H):
                b = nbank()
                proj_chunk(Wv, wbuf, h, b)
                rope_chunk(b, QT0 + h, h % 2)
            ring_done(wi)
            stage('q')
            wi, wbuf = ring_next(l, "k")
            Wv = WR[:, wbuf, 0:4096].rearrange("p (k n) -> p k n", k=KC)
            for h in range(H):
                b = nbank()
                proj_chunk(Wv, wbuf, h, b)
                rope_chunk(b, KT0 + h, h % 2)
            ring_done(wi)
            stage('k')
            wi, wbuf = ring_next(l, "v")
            Wv = WR[:, wbuf, 0:4096].rearrange("p (k n) -> p k n", k=KC)
            for c in range(NCH):
                b = nbank()
                for kc in range(KC):
                    op("pe", "matmul", PS[:, b, :], lhsT=Hh[:, kc, c * 128:(c + 1) * 128], rhs=Wv[:, kc, :],
                       start=(kc == 0), stop=(kc == KC - 1), r=[("W", wbuf), ("H", kc)], w=[PSp(b)])
                op("act", "activation", out=AR[:, VT0 + c, :], in_=PS[:, b, :], func=AF.Copy,
                   r=[PSp(b)], w=[ARp(VT0 + c)])
            ring_done(wi)
            stage('v')
            wi, wbuf = ring_next(l, "gr")
            Wv = WR[:, wbuf, 0:4096].rearrange("p (k n) -> p k n", k=KC)
            for h in range(H):
                b = nbank()
                proj_chunk(Wv, wbuf, h, b)
                op("act", "activation", out=AR[:, SG0 + h, :], in_=PS[:, b, :], func=AF.Silu,
                   r=[PSp(b)], w=[ARp(SG0 + h)])
            ring_done(wi)
            stage('gr')
            wi, wbuf = ring_next(l, "ux")
            Wv = WR[:, wbuf, 0:4096].rearrange("p (k n) -> p k n", k=KC)
            op("pool", "tensor_copy", out=U[:, :, 0:15], in_=HALO[:, l, :, :], r=[("HALO", l)], w=[("U", 0)])
            for c in range(2):
                b = nbank()
                proj_chunk(Wv, wbuf, c, b)
                op("dve", "tensor_copy", out=U[:, c, 15:15 + T], in_=PS[:, b, :], r=[PSp(b)], w=[("U", 0)])
            for c in range(2):
                b = nbank()
                proj_chunk(Wv, wbuf, 2 + c, b)
                op("act", "activation", out=AR[:, QX0 + c, :], in_=PS[:, b, :], func=AF.Copy,
                   r=[PSp(b)], w=[ARp(QX0 + c)])
            ring_done(wi)

            stage('ux')
            maskT = CST[:, C_MASK:C_MASK + 512]
            ksc = CST[:, C_KSC:C_KSC + 512]
            gct = CST[:, C_GC:C_GC + 512]
            epsr = CST[:, C_EPSR:C_EPSR + 512]
            for c in range(NCH):
                cl = slice(c * 128, (c + 1) * 128)
                bs = nbank()
                for h in range(H):
                    op("pe", "matmul", PS[:, bs, h * 128:(h + 1) * 128], lhsT=AR[:, KT0 + h, cl], rhs=AR[:, QT0 + h, cl],
                       start=True, stop=True, r=[ARp(KT0 + h), ARp(QT0 + h)], w=[PSp(bs)])
                si = c % 2
                op("dve", "tensor_tensor", out=ST[:, si, :], in0=PS[:, bs, :], in1=maskT, op=ALU.mult,
                   r=[PSp(bs), ("CST", 0)], w=[("ST", si)])
                pb = nptb()
                for h in range(H):
                    op("pe", "transpose", PT[:, pb, h * 128:(h + 1) * 128], AR[:, KT0 + h, cl], ident,
                       r=[ARp(KT0 + h), ("CB", 0)], w=[("PT", pb)])
                op("dve", "tensor_tensor", out=KTOK[:, si, :], in0=PT[:, pb, :], in1=ksc, op=ALU.mult,
                   r=[("PT", pb), ("CST", 0)], w=[("KTOK", si)])
                bo = nbank()
                for h in range(H):
                    hs = slice(h * 128, (h + 1) * 128)
                    op("pe", "matmul", PS[:, bo, hs], lhsT=AR[:, VT0 + c, hs], rhs=ST[:, si, hs],
                       start=True, stop=False, r=[ARp(VT0 + c), ("ST", si)], w=[PSp(bo)])
                    op("pe", "matmul", PS[:, bo, hs], lhsT=RBF[:, l, hs], rhs=AR[:, QT0 + h, cl],
                       start=False, stop=True, r=[("RBF", l), ARp(QT0 + h)], w=[PSp(bo)])
                bk = nbank()
                for h in range(H):
                    hs = slice(h * 128, (h + 1) * 128)
                    op("pe", "matmul", PS[:, bk, hs], lhsT=KTOK[:, si, hs], rhs=AR[:, VT0 + c, hs],
                       start=True, stop=True, r=[("KTOK", si), ARp(VT0 + c)], w=[PSp(bk)])
                op("dve", "tensor_tensor", out=Y1[:], in0=R32[:, l, :], in1=PS[:, bk, :], op=ALU.add,
                   r=[("R32", l), PSp(bk)], w=[("Y1", 0)])
                op("pool", "tensor_tensor", out=RBF[:, l, :], in0=Y1[:], in1=gct, op=ALU.mult,
                   r=[("Y1", 0), ("CST", 0)], w=[("RBF", l)])
                op("dve", "tensor_tensor", out=R32[:, l, :], in0=Y1[:], in1=gct, op=ALU.mult,
                   r=[("Y1", 0), ("CST", 0)], w=[("R32", l)])
                op("act", "activation", out=YSQ[:, si, :], in_=PS[:, bo, :], func=AF.Square,
                   r=[PSp(bo)], w=[("YSQ", si)])
                bn = nbank()
                op("pe", "matmul", PS[:, bn, :], lhsT=ones, rhs=YSQ[:, si, :], start=True, stop=True,
                   r=[("YSQ", si), ("CB", 0)], w=[PSp(bn)])
                op("dve", "scalar_tensor_tensor", out=TT[:], in0=PS[:, bn, :], scalar=1.0 / 128, in1=epsr,
                   op0=ALU.mult, op1=ALU.add, r=[PSp(bn), ("CST", 0)], w=[("TT", 0)])
                op("act", "activation", out=TT[:], in_=TT[:], func=AF.Ln, r=[("TT", 0)], w=[("TT", 0)])
                op("act", "activation", out=HR[:], in_=TT[:], func=AF.Exp, scale=-0.5, r=[("TT", 0)], w=[("HR", 0)])
                op("dve", "tensor_tensor", out=RA[:], in0=PS[:, bo, :], in1=HR[:], op=ALU.mult,
                   r=[PSp(bo), ("HR", 0)], w=[("RA", 0)])
                op("pool", "tensor_tensor", out=AR[:, YR0:YR0 + 4, cl],
                   in0=RA[:].rearrange("p (h i) -> p h i", h=H), in1=AR[:, SG0:SG0 + 4, cl], op=ALU.mult,
                   r=[("RA", 0)] + [ARp(SG0 + h) for h in range(H)], w=[ARp(YR0 + h) for h in range(H)])

            stage('ret')
            op("pool", "tensor_tensor", out=PB[:, :, 1:T + 15], in0=U[:, :, 1:T + 15], in1=U[:, :, 0:T + 14], op=ALU.add,
               r=[("U", 0)], w=[("PB", 0)])
            op("pool", "tensor_tensor", out=PC[64:128, 0, 3:T + 15], in0=PB[64:128, 0, 3:T + 15], in1=PB[64:128, 0, 1:T + 13],
               op=ALU.add, r=[("PB", 0)], w=[("PC", 0)])
            op("pool", "tensor_tensor", out=PC[:, 1, 3:T + 15], in0=PB[:, 1, 3:T + 15], in1=PB[:, 1, 1:T + 13],
               op=ALU.add, r=[("PB", 0)], w=[("PC", 0)])
            op("pool", "tensor_tensor", out=PB[:, 1, 7:T + 15], in0=PC[:, 1, 7:T + 15], in1=PC[:, 1, 3:T + 11],
               op=ALU.add, r=[("PC", 0)], w=[("PB", 0)])
            op("pool", "tensor_tensor", out=PC[64:128, 1, 15:T + 15], in0=PB[64:128, 1, 15:T + 15], in1=PB[64:128, 1, 7:T + 7],
               op=ALU.add, r=[("PB", 0)], w=[("PC", 0)])
            wins = [(PB, 0, 0), (PC, 0, 64), (PB, 1, 0), (PC, 1, 64)]
            for g, (buf, c, p0) in enumerate(wins):
                bn_ = "PB" if buf is PB else "PC"
                if t <= 1:
                    op("pool", "tensor_tensor", out=buf[p0:p0 + 64, c, 15:31], in0=buf[p0:p0 + 64, c, 15:31],
                       in1=CST[p0:p0 + 64, C_CORR + t * 32 + c * 16:C_CORR + t * 32 + c * 16 + 16], op=ALU.mult,
                       r=[(bn_, 0), ("CST", 0)], w=[(bn_, 0)])
                op("dve", "scalar_tensor_tensor", out=POOLED[p0:p0 + 64, c, :], in0=buf[p0:p0 + 64, c, 15:T + 15],
                   scalar=CST[p0:p0 + 64, C_INVW + c:C_INVW + c + 1], in1=U[p0:p0 + 64, c, 15:T + 15],
                   op0=ALU.mult, op1=ALU.subtract, r=[(bn_, 0), ("U", 0), ("CST", 0)], w=[("POOLED", c)])
            op("pool", "tensor_copy", out=HALO[:, l, :, :], in_=U[:, :, T:T + 15], r=[("U", 0)], w=[("HALO", l)])
            for c in range(2):
                b = nbank()
                op("pe", "matmul", PS[:, b, :], lhsT=WMIX[:, (l * 2 + c) * 128:(l * 2 + c + 1) * 128], rhs=POOLED[:, c, :],
                   start=True, stop=True, r=[("WMIX", 0), ("POOLED", c)], w=[PSp(b)])
                op("act", "activation", out=YPOOL[:, c, :], in_=PS[:, b, :], func=AF.Copy, scale=gcol(l, "ps", c),
                   r=[PSp(b), ("GAINS", 0)], w=[("YPOOL", c)])

            stage('pool')
            for hc in range(2):
                ba = nbank()
                bd = nbank()
                for hh in range(2):
                    h = 2 * hc + hh
                    r0 = hh * 64
                    eb = h % 2
                    bx = [nbank(), nbank()]
                    for mc in range(2):
                        op("pe", "matmul", PS[:, bx[mc], :], lhsT=MK[r0:r0 + 64, l, hc, mc * 128:(mc + 1) * 128],
                           rhs=AR[r0:r0 + 64, QX0 + hc, :], start=True, stop=True,
                           r=[("MK", l), ARp(QX0 + hc)], w=[PSp(bx[mc])])
                    for mc in range(2):
                        op("act", "activation", out=EXPT[:, eb, mc, :], in_=PS[:, bx[mc], :], func=AF.Exp, scale=0.125,
                           r=[PSp(bx[mc])], w=[("EXPT", eb)])
                    for mc in range(2):
                        first = (hh == 0 and mc == 0)
                        last = (hh == 1 and mc == 1)
                        op("pe", "matmul", PS[:, ba, :], lhsT=MV[:, l, mc, h, :], rhs=EXPT[:, eb, mc, :],
                           start=first, stop=last, r=[("MV", l), ("EXPT", eb)], w=[PSp(ba)])
                        op("pe", "matmul", PS[:, bd, :], lhsT=CB[:, B_OPAD + hh * 128:B_OPAD + (hh + 1) * 128],
                           rhs=EXPT[:, eb, mc, :], start=first, stop=last, r=[("CB", 0), ("EXPT", eb)], w=[PSp(bd)])
                op("dve", "reciprocal", out=RDEN[:], in_=PS[:, bd, :], r=[PSp(bd)], w=[("RDEN", 0)])
                op("dve", "tensor_tensor", out=YX[:, hc, :], in0=PS[:, ba, :], in1=RDEN[:], op=ALU.mult,
                   r=[PSp(ba), ("RDEN", 0)], w=[("YX", hc)])

            stage('xattn')
            for c in range(8):
                wi, wbuf = ring_next(l, f"mg{c}")
                Wg = WR[:, wbuf, 0:3072].rearrange("p (k n) -> p k n", k=KC)
                Wr = WR[:, wbuf, 3072:3584].rearrange("p (k n) -> p k n", k=4)
                Wp = WR[:, wbuf, 3584:3840].rearrange("p (k n) -> p k n", k=2)
                Wx = WR[:, wbuf, 3840:4096].rearrange("p (k n) -> p k n", k=2)
                ts = (c % 2) * 3
                bg = []
                for j in range(3):
                    b = nbank()
                    bg.append(b)
                    proj_chunk(Wg, wbuf, j, b)
                    op("act", "activation", out=TG[:, ts + j, :], in_=PS[:, b, :], func=AF.Tanh, scale=0.5,
                       r=[PSp(b)], w=[("TG", ts + j)])
                br = nbank()
                for h in range(H):
                    op("pe", "matmul", PS[:, br, :], lhsT=Wr[:, h, :], rhs=AR[:, YR0 + h, :], start=(h == 0), stop=(h == H - 1),
                       r=[("W", wbuf), ARp(YR0 + h)], w=[PSp(br)])
                bp = nbank()
                for j in range(2):
                    op("pe", "matmul", PS[:, bp, :], lhsT=Wp[:, j, :], rhs=YPOOL[:, j, :], start=(j == 0), stop=(j == 1),
                       r=[("W", wbuf), ("YPOOL", j)], w=[PSp(bp)])
                bxx = nbank()
                for j in range(2):
                    op("pe", "matmul", PS[:, bxx, :], lhsT=Wx[:, j, :], rhs=YX[:, j, :], start=(j == 0), stop=(j == 1),
                       r=[("W", wbuf), ("YX", j)], w=[PSp(bxx)])
                ring_done(wi)
                for j, bb in enumerate((br, bp, bxx)):
                    op("dve", "scalar_tensor_tensor", out=Mm[:, ts + j, :], in0=TG[:, ts + j, :], scalar=1.0, in1=PS[:, bb, :],
                       op0=ALU.add, op1=ALU.mult, r=[("TG", ts + j), PSp(bb)], w=[("Mm", ts + j)])
                op("pool", "tensor_tensor", out=Mm[:, ts, :], in0=Mm[:, ts, :], in1=Mm[:, ts + 1, :], op=ALU.add,
                   r=[("Mm", ts), ("Mm", ts + 1)], w=[("Mm", ts)])
                op("pool", "tensor_tensor", out=AR[:, c, :], in0=Mm[:, ts, :], in1=Mm[:, ts + 2, :], op=ALU.add,
                   r=[("Mm", ts), ("Mm", ts + 2)], w=[ARp(c)])

            stage('merge')
            for s in range(2):
                wi, wbuf = ring_next(l, f"wo{s}")
                Wv = WR[:, wbuf, 0:4096].rearrange("p (k n) -> p k n", k=KC)
                for j in range(4):
                    oc = s * 4 + j
                    b = nbank()
                    for kc in range(KC):
                        op("pe", "matmul", PS[:, b, :], lhsT=Wv[:, kc, j * 128:(j + 1) * 128], rhs=AR[:, kc, :],
                           start=(kc == 0), stop=(kc == KC - 1), r=[("W", wbuf), ARp(kc)], w=[PSp(b)])
                    op("dve", "scalar_tensor_tensor", out=X[:, oc, :], in0=PS[:, b, :], scalar=0.5, in1=X[:, oc, :],
                       op0=ALU.mult, op1=ALU.add, r=[PSp(b), ("X", oc)], w=[("X", oc)])
                    stats_acc(oc)
                ring_done(wi)

        def ffn(t, l):
            norm_to_h(l, "ffn")
            for s in range(11):
                wi, wbuf = ring_next(l, f"fi{s}")
                Wv = WR[:, wbuf, 0:4096].rearrange("p (k n) -> p k n", k=KC)
                for j in range(2):
                    c = 2 * s + j
                    ba = nbank()
                    proj_chunk(Wv, wbuf, j, ba)
                    bb = nbank()
                    proj_chunk(Wv, wbuf, 2 + j, bb)
                    ti = c % 2
                    op("act", "activation", out=TH[:, ti, :], in_=PS[:, ba, :], func=AF.Silu,
                       r=[PSp(ba)], w=[("TH", ti)])
                    op("dve", "tensor_tensor", out=AR[:, c, :], in0=TH[:, ti, :], in1=PS[:, bb, :], op=ALU.mult,
                       r=[("TH", ti), PSp(bb)], w=[ARp(c)])
                ring_done(wi)
            for oc in range(8):
                wi, wbuf = ring_next(l, f"fo{oc}")
                Wv = WR[:, wbuf, 0:2816].rearrange("p (k n) -> p k n", k=FKC)
                b = nbank()
                for kc in range(FKC):
                    op("pe", "matmul", PS[:, b, :], lhsT=Wv[:, kc, :], rhs=AR[:, kc, :],
                       start=(kc == 0), stop=(kc == FKC - 1), r=[("W", wbuf), ARp(kc)], w=[PSp(b)])
                op("dve", "tensor_tensor", out=X[:, oc, :], in0=PS[:, b, :], in1=X[:, oc, :], op=ALU.add,
                   r=[PSp(b), ("X", oc)], w=[("X", oc)])
                stats_acc(oc)
                ring_done(wi)

        xT_v = xT_d.rearrange("(kc p) s -> p kc s", p=128)
        out_v = out_d.rearrange("(kc p) s -> p kc s", p=128)
        last_store = None
        groups = [[2 * i, 2 * i + 1] for i in range(npair)]
        Xparts = [("X", kc) for kc in range(KC)]
        for t in range(NT):
            op("sp", "dma_start", out=X[:], in_=xT_v[:, :, t * T:(t + 1) * T], w=Xparts, dma=xsem)
            if t >= 1:
                for kc in range(KC):
                    si = kc % 2
                    op("sp", "dma_start", out=STG[:, si, :], in_=rcv_d[0:128, kc * T:(kc + 1) * T],
                       r=[("RCV", 0)], w=[("STG", si)], dma=stgsem[si])
                    op("dve", "scalar_tensor_tensor", out=X[:, kc, :], in0=STG[:, si, :], scalar=cs(C_SG),
                       in1=X[:, kc, :], op0=ALU.mult, op1=ALU.add,
                       r=[("STG", si), ("X", kc), ("CST", 0)], w=[("X", kc)])
            for kc in range(KC):
                stats_acc(kc)
            rope_tables(t)
            for l in range(DEPTH):
                mixer(t, l)
                ffn(t, l)
            if t == 0:
                op("pool", "tensor_scalar", out=R32[:], in0=R32[:], scalar1=cs(C_KEEP), scalar2=None, op0=ALU.mult,
                   r=[("R32", l) for l in range(DEPTH)] + [("CST", 0)], w=[("R32", l) for l in range(DEPTH)])
                op("pool", "tensor_scalar", out=RBF[:], in0=RBF[:], scalar1=cs(C_KEEP), scalar2=None, op0=ALU.mult,
                   r=[("RBF", l) for l in range(DEPTH)] + [("CST", 0)], w=[("RBF", l) for l in range(DEPTH)])
                op("pool", "tensor_scalar", out=HALO[:], in0=HALO[:], scalar1=cs(C_KEEP), scalar2=None, op0=ALU.mult,
                   r=[("HALO", l) for l in range(DEPTH)] + [("CST", 0)], w=[("HALO", l) for l in range(DEPTH)])
            if t < NT - 1:
                op("sp", "dma_start", out=snd_d.rearrange("p (k n) -> p k n", k=KC), in_=X[:],
                   r=Xparts, w=[("SND", 0)], dma=sndsem)
                op("pool", "collective_compute", "AllGather", ALU.bypass, replica_groups=groups,
                   ins=[snd_d], outs=[rcv_d], r=[("SND", 0)], w=[("RCV", 0)], dma=ccsem, inc=1)
            stats_finish()
            for kc in range(KC):
                op("dve", "scalar_tensor_tensor", out=X[:, kc, :], in0=X[:, kc, :], scalar=gfin(kc),
                   in1=RSTD[:], op0=ALU.mult, op1=ALU.mult,
                   r=[("X", kc), ("RSTD", 0), ("GAINS", 0)], w=[("X", kc)])
            last_store = op("sp", "dma_start", out=out_v[:, :, t * T:(t + 1) * T], in_=X[:], r=Xparts, dma=osem)
        if stopped[0]:
            stopped[0] = False
            last_store = op("sp", "dma_start", out=out_v[:, :, 0:T], in_=X[:],
                            r=[("X", kc) for kc in range(KC)] + [("RSTD", 0)], dma=osem)
        else:
            assert ring["cur"] == len(slab_list)

        block = es.enter_context(nc.Block())

        @block.sync
        def _(e):
            sc.emit("sp", e, esem)
            e.wait_ge(osem, last_store.val)

        @block.tensor
        def _(e):
            sc.emit("pe", e, esem)

        @block.scalar
        def _(e):
            sc.emit("act", e, esem)

        @block.vector
        def _(e):
            sc.emit("dve", e, esem)

        @block.gpsimd
        def _(e):
            sc.emit("pool", e, esem)

    return nc


def _slab(Wm, cols):
    K = Wm.shape[0]
    sub = Wm[:, cols]
    return np.ascontiguousarray(sub.reshape(K // 128, 128, -1).transpose(1, 0, 2)).reshape(128, -1)


def _layer_line(l, w_in, w_up_ret, w_up_pool, w_up_x, w_out, w_ffn_in, w_ffn_out, w_mem_kv):
    ar = np.arange
    parts = {}
    parts["memkv"] = _slab(w_mem_kv[l], ar(512))
    parts["q"] = _slab(w_in[l], ar(0, 512))
    parts["k"] = _slab(w_in[l], ar(512, 1024))
    parts["v"] = _slab(w_in[l], ar(1024, 1536))
    parts["gr"] = _slab(w_in[l], ar(1536, 2048))
    parts["ux"] = _slab(w_in[l], ar(2048, 2560))
    for c in range(8):
        gcols = np.concatenate([2560 + j * 1024 + c * 128 + ar(128) for j in range(3)])
        oc = c * 128 + ar(128)
        parts[f"mg{c}"] = np.concatenate(
            [_slab(w_in[l], gcols), _slab(w_up_ret[l], oc), _slab(w_up_pool[l], oc), _slab(w_up_x[l], oc)], axis=1)
    for s in range(2):
        parts[f"wo{s}"] = _slab(w_out[l], ar(s * 512, (s + 1) * 512))
    for s in range(11):
        cols = np.concatenate([(2 * s) * 128 + ar(128), (2 * s + 1) * 128 + ar(128),
                               FH + (2 * s) * 128 + ar(128), FH + (2 * s + 1) * 128 + ar(128)])
        parts[f"fi{s}"] = _slab(w_ffn_in[l], cols)
    for j in range(8):
        parts[f"fo{j}"] = _slab(w_ffn_out[l], ar(j * 128, (j + 1) * 128))
    line = np.concatenate([parts[n] for n, _ in SLABS], axis=1)
    assert line.shape == (128, LINE), line.shape
    return line


def _consts(stage):
    c = np.zeros((128, NCONST), np.float64)
    g = 1.0 - np.exp2(-5.0 - np.arange(H))
    j = np.arange(128)
    dk = 128.0 ** -0.5
    for h in range(H):
        ginv = g[h] ** (-(j + 1.0))
        m = (j[:, None] <= j[None, :]) * (ginv[:, None] * dk)
        c[:, C_MASK + h * 128:C_MASK + (h + 1) * 128] = m
        c[:, C_KSC + h * 128:C_KSC + (h + 1) * 128] = (ginv * dk)[:, None]
        c[:, C_GC + h * 128:C_GC + (h + 1) * 128] = g[h] ** 128.0
        c[:, C_EPSR + h * 128:C_EPSR + (h + 1) * 128] = (EPS * g[h] ** (-2.0 * (j + 1.0)))[None, :]
    wins = (2, 4, 8, 16)
    for ch in range(2):
        for half in range(2):
            w = wins[ch * 2 + half]
            p = slice(half * 64, half * 64 + 64)
            c[p, C_INVW + ch] = 1.0 / w
            tt = np.arange(16)
            real = (w / np.minimum(tt + 1, w))[None, :]
            for st in range(2):
                val = real if st == stage else 1.0
                c[p, C_CORR + st * 32 + ch * 16:C_CORR + st * 32 + ch * 16 + 16] = val
    inv_freq = (10000.0 ** (-np.arange(0, 128, 2, dtype=np.float32) / np.float32(128))).astype(np.float32)
    c[:, C_INVF] = np.concatenate([inv_freq, inv_freq])
    c[0:64, C_SGN] = -1.0
    c[64:128, C_SGN] = 1.0
    c[:, C_EPS] = EPS
    c[:, C_SG] = float(stage)
    c[:, C_KEEP] = 1.0 - float(stage)
    cb = np.zeros((128, NCB), np.float32)
    cb[:, B_ID:B_ID + 128] = np.eye(128)
    pm = np.zeros((128, 128), np.float32)
    mm = np.arange(128)
    pm[(mm + 64) % 128, mm] = 1.0
    cb[:, B_PERM:B_PERM + 128] = pm
    cb[:, B_ONES:B_ONES + 128] = 1.0
    cb[:, B_OPAD + 0:B_OPAD + 64] = 1.0
    cb[:, B_OPAD + 128 + 64:B_OPAD + 256] = 1.0
    return c.astype(np.float32), cb


_PROG_CACHE = {}


def kernel(x, mem, positions, norm_mix, w_in, w_up_ret, w_pool_mix, pool_scale, w_up_pool,
           norm_mem, w_mem_kv, w_up_x, w_out, norm_ffn, w_ffn_in, w_ffn_out, final_norm):
    x = np.asarray(x, np.float32)
    mem = np.asarray(mem, np.float32)
    B, S, _ = x.shape
    DEPTH = int(np.asarray(norm_mix).shape[0])
    f = lambda a: np.asarray(a, np.float32)
    w_in, w_up_ret, w_up_pool, w_up_x, w_out = f(w_in), f(w_up_ret), f(w_up_pool), f(w_up_x), f(w_out)
    w_ffn_in, w_ffn_out, w_mem_kv, w_pool_mix = f(w_ffn_in), f(w_ffn_out), f(w_mem_kv), f(w_pool_mix)
    norm_mix, norm_ffn, norm_mem, pool_scale, final_norm = f(norm_mix), f(norm_ffn), f(norm_mem), f(pool_scale), f(final_norm)

    LD = DEPTH // 2
    NT = S // T
    SP_ = (NT + 1) * T
    lines = [_layer_line(l, w_in, w_up_ret, w_up_pool, w_up_x, w_out, w_ffn_in, w_ffn_out, w_mem_kv)
             for l in range(DEPTH)]
    pos = np.asarray(positions, np.int32).reshape(S)

    def stage_inputs(stage):
        ls = list(range(stage * LD, (stage + 1) * LD))
        wf = np.stack([lines[l] for l in ls], axis=0)
        cst, cstb = _consts(stage)
        gains = np.zeros((128, LD * 26 + 8), np.float32)
        wmix = np.zeros((128, LD, 2, 128), np.float32)
        for i, l in enumerate(ls):
            gains[:, i * 26 + 0:i * 26 + 8] = norm_mix[l].reshape(8, 128).T
            gains[:, i * 26 + 8:i * 26 + 16] = norm_ffn[l].reshape(8, 128).T
            gains[:, i * 26 + 16:i * 26 + 24] = norm_mem[l].reshape(8, 128).T
            gains[:, i * 26 + 24:i * 26 + 26] = pool_scale[l].reshape(2, 128).T
            for g in range(4):
                c, half = g // 2, g % 2
                wmix[half * 64:half * 64 + 64, i, c, half * 64:half * 64 + 64] = w_pool_mix[l, g]
        gains[:, LD * 26:LD * 26 + 8] = final_norm.reshape(8, 128).T
        posp = np.zeros((1, SP_), np.int32)
        posp[0, stage * T:stage * T + S] = pos
        return dict(wf=wf, cst=cst, cstb=cstb, gains=gains, wmix=wmix.reshape(128, -1), pos=posp)

    st_in = [stage_inputs(0), stage_inputs(1)]
    key = (S, LD, B)
    if key not in _PROG_CACHE:
        _PROG_CACHE[key] = build_program(S, LD, B)
    nc = _PROG_CACHE[key]
    in_maps = []
    for b in range(B):
        memT = np.ascontiguousarray(mem[b].T)
        xa = np.zeros((D, SP_), np.float32)
        xa[:, 0:S] = x[b].T
        in_maps.append(dict(st_in[0], xT=xa, memT=memT))
        in_maps.append(dict(st_in[1], xT=np.zeros((D, SP_), np.float32), memT=memT))
    ncores = 2 * B
    res = run_bass_kernel_spmd(nc, in_maps, core_ids=list(range(ncores)))
    out = np.stack([np.ascontiguousarray(res.results[2 * b + 1]["outT"][:, T:T + S].T) for b in range(B)], axis=0)
    return out.astype(np.float32)
```

```python
import contextlib
import numpy as np
import concourse.bass as bass
import concourse.mybir as mybir
from concourse.bass_utils import run_bass_kernel_spmd

F32, BF16, I32 = mybir.dt.float32, mybir.dt.bfloat16, mybir.dt.int32
ALU = mybir.AluOpType
AF = mybir.ActivationFunctionType

D = 1024
KC = 8
T = 512
NCH = T // 128
H = 4
MEM = 256
FH = 2816
FKC = 22
IN_W = 5632
EPS = 1e-6
NB = 5
SLABW = 4096

SLABS = [("memkv", 4096), ("ux", 4096), ("q", 4096), ("k", 4096), ("v", 4096), ("gr", 4096)]
SLABS += [(f"mg{c}", 4096) for c in range(8)]
SLABS += [("wo0", 4096), ("wo1", 4096)]
SLABS += [(f"fi{s}", 4096) for s in range(11)]
SLABS += [(f"fo{j}", 2816) for j in range(8)]
SLAB_OFF = {}
_o = 0
for _n, _w in SLABS:
    SLAB_OFF[_n] = (_o, _w)
    _o += _w
LINE = _o

C_MASK = 0
C_KSC = 512
C_GC = 1024
C_EPSR = 1536
C_INVW = 2048
C_CORR = 2050
C_INVF = 2114
C_SGN = 2115
C_EPS = 2116
C_SG = 2117
C_KEEP = 2118
NCONST = 2120
B_ID = 0
B_PERM = 128
B_ONES = 256
B_OPAD = 384
NCB = 640


class Op:
    __slots__ = ("eng", "fn", "waits", "idx", "needed", "dma", "val", "inc")


class Sched:
    ENGS = ("pe", "act", "dve", "pool", "sp")

    def __init__(self):
        self.streams = {e: [] for e in self.ENGS}
        self.last_w = {}
        self.readers = {}
        self.known = {e: {} for e in self.ENGS}
        self.dma_count = {}

    def add(self, eng, fn, reads=(), writes=(), dma=None, inc=16):
        op = Op()
        op.eng, op.fn, op.needed, op.dma, op.val = eng, fn, False, dma, None
        op.inc = inc
        st = self.streams[eng]
        op.idx = len(st)
        deps = []
        for p in reads:
            w = self.last_w.get(p)
            if w is not None:
                deps.append((w, True))
            if p[0] in ("PS", "PT"):
                rd = self.readers.get(p)
                if rd:
                    for k_, r in rd.items():
                        if k_ != eng:
                            deps.append((r, True))
        for p in writes:
            w = self.last_w.get(p)
            if w is not None:
                deps.append((w, True))
            rd = self.readers.get(p)
            if rd:
                for r in rd.values():
                    deps.append((r, False))
        waits = {}
        kn = self.known[eng]
        for d, hazard_rw in deps:
            if d is op:
                continue
            if d.dma is not None:
                key = ("dma", id(d.dma))
                if kn.get(key, 0) >= d.val:
                    continue
                kn[key] = d.val
                waits[key] = d
                continue
            if d.eng == eng:
                if eng == "pe" or eng == "sp":
                    continue
            key = d.eng
            if kn.get(key, -1) >= d.idx:
                continue
            kn[key] = d.idx
            d.needed = True
            prev = waits.get(key)
            if prev is None or prev.idx < d.idx:
                waits[key] = d
        op.waits = list(waits.values())
        if dma is not None:
            c = self.dma_count.get(id(dma), 0) + 1
            self.dma_count[id(dma)] = c
            op.val = inc * c
        st.append(op)
        for p in reads:
            self.readers.setdefault(p, {})[eng if dma is None else ("dma", id(op))] = op
        for p in writes:
            self.last_w[p] = op
            self.readers[p] = {}
        return op

    def mark_written(self, parts, op):
        for p in parts:
            self.last_w[p] = op
            self.readers[p] = {}

    def emit(self, eng, e, esem):
        if not hasattr(self, "_vals"):
            self._vals = True
            for en in self.ENGS:
                c = 0
                for op in self.streams[en]:
                    if op.dma is None and op.needed:
                        c += 1
                        op.val = c
        for op in self.streams[eng]:
            for d in op.waits:
                if d.dma is not None:
                    e.wait_ge(d.dma, d.val)
                else:
                    e.wait_ge(esem[d.eng], d.val)
            ins = op.fn(e)
            if op.dma is not None:
                ins.then_inc(op.dma, op.inc)
            elif op.needed:
                ins.then_inc(esem[eng], 1)


class _Stop(Exception):
    pass


def build_program(S, DEPTH, npair=4, stop=None):
    NT = S // T + 1
    SP_ = NT * T
    stage_ctr = [0]
    stopped = [False]

    def stage(name):
        stage_ctr[0] += 1
        if stop is not None and stage_ctr[0] >= stop:
            if not stopped[0]:
                print('STOP at stage', stage_ctr[0], name)
            stopped[0] = True
    nc = bass.Bass("TRN2", target_bir_lowering=False)
    xT_d = nc.dram_tensor("xT", [D, SP_], F32, kind="ExternalInput").ap()
    memT_d = nc.dram_tensor("memT", [D, MEM], F32, kind="ExternalInput").ap()
    pos_d = nc.dram_tensor("pos", [1, SP_], I32, kind="ExternalInput").ap()
    wf_d = nc.dram_tensor("wf", [DEPTH, 128, LINE], F32, kind="ExternalInput").ap()
    cst_d = nc.dram_tensor("cst", [128, NCONST], F32, kind="ExternalInput").ap()
    cstb_d = nc.dram_tensor("cstb", [128, NCB], F32, kind="ExternalInput").ap()
    gains_d = nc.dram_tensor("gains", [128, DEPTH * 26 + 8], F32, kind="ExternalInput").ap()
    wmix_d = nc.dram_tensor("wmix", [128, DEPTH * 2 * 128], F32, kind="ExternalInput").ap()
    out_d = nc.dram_tensor("outT", [D, SP_], F32, kind="ExternalOutput").ap()
    snd_d = nc.dram_tensor("snd", [128, KC * T], F32, kind="Internal").ap()
    rcv_d = nc.dram_tensor("rcv", [256, KC * T], F32, kind="Internal", addr_space="Local").ap()
    wb_d = nc.dram_tensor("wb", [DEPTH, 128, LINE], BF16, kind="Internal").ap()

    es = contextlib.ExitStack()
    with es:
        def sb(name, shape, dt):
            return es.enter_context(nc.sbuf_tensor(name, shape, dt))

        def sem(name):
            return es.enter_context(nc.semaphore(name))

        X = sb("X", [128, KC, T], F32)
        Hh = sb("Hh", [128, KC, T], BF16)
        SQ = sb("SQ", [128, 2, T], BF16)
        RSTD = sb("RSTD", [128, T], F32)
        AR = sb("AR", [128, 22, T], BF16)
        RA = sb("RA", [128, T], F32)
        RB = sb("RB", [128, T], F32)
        QB = sb("QB", [128, 2, T], BF16)
        U = sb("U", [128, 2, T + 15], F32)
        PB = sb("PB", [128, 2, T + 15], F32)
        PC = sb("PC", [128, 2, T + 15], F32)
        POOLED = sb("POOLED", [128, 2, T], BF16)
        YPOOL = sb("YPOOL", [128, 2, T], BF16)
        EXPT = sb("EXPT", [128, 2, 2, T], BF16)
        YX = sb("YX", [128, 2, T], BF16)
        RDEN = sb("RDEN", [128, T], F32)
        ST = sb("ST", [128, 2, T], BF16)
        KTOK = sb("KTOK", [128, 2, T], BF16)
        YSQ = sb("YSQ", [128, 2, T], BF16)
        TT = sb("TT", [128, T], F32)
        HR = sb("HR", [128, T], F32)
        Y1 = sb("Y1", [128, T], F32)
        TG = sb("TG", [128, 6, T], F32)
        Mm = sb("Mm", [128, 6, T], F32)
        TH = sb("TH", [128, 2, T], F32)
        R32 = sb("R32", [128, DEPTH, T], F32)
        RBF = sb("RBF", [128, DEPTH, T], BF16)
        HALO = sb("HALO", [128, DEPTH, 2, 15], F32)
        MK = sb("MK", [128, DEPTH, 2, MEM], BF16)
        MV = sb("MV", [128, DEPTH, 2, 4, 128], BF16)
        WR = sb("WR", [128, NB, SLABW], BF16)
        COS = sb("COS", [128, T], F32)
        SIN = sb("SIN", [128, T], F32)
        POSI = sb("POSI", [128, T], I32)
        STG = sb("STG", [128, 2, T], F32)
        CST = sb("CST", [128, NCONST], F32)
        CB = sb("CB", [128, NCB], BF16)
        GAINS = sb("GAINS", [128, DEPTH * 26 + 8], F32)
        WMIX = sb("WMIX", [128, DEPTH * 2 * 128], BF16)
        PS = es.enter_context(nc.psum_tensor("PS", [128, 7, T], F32))
        PT = es.enter_context(nc.psum_tensor("PT", [128, 2, T], BF16))

        esem = {e: sem("s_" + e) for e in ("pe", "act", "dve", "pool")}
        wsem = [sem(f"w{b}") for b in range(NB)]
        castsem = [sem(f"cast{l}") for l in range(DEPTH)]
        xsem = sem("xld")
        possem = sem("posld")
        osem = sem("ost")
        csem = sem("cld")
        csem2 = sem("cld2")
        stgsem = [sem("stg0"), sem("stg1")]
        sndsem = sem("snd")
        ccsem = sem("cc")

        sc = Sched()

        def op(eng, method, *args, r=(), w=(), dma=None, inc=16, **kw):
            if stopped[0]:
                return None
            return sc.add(eng, lambda e: getattr(e, method)(*args, **kw), r, w, dma, inc)

        bank_ctr = [0]

        reserved = set()

        def nbank():
            while True:
                b = bank_ctr[0]
                bank_ctr[0] = (b + 1) % 7
                if b not in reserved:
                    return b

        ptc = [0]

        def nptb():
            b = ptc[0]
            ptc[0] = 1 - b
            return b

        def PSp(b):
            return ("PS", b)

        def cs(a, n=1):
            return CST[:, a:a + n]

        ident = CB[:, B_ID:B_ID + 128]
        perm = CB[:, B_PERM:B_PERM + 128]
        ones = CB[:, B_ONES:B_ONES + 128]

        def gcol(l, kind, i):
            base = l * 26 + {"mix": 0, "ffn": 8, "mem": 16, "ps": 24}[kind]
            return GAINS[:, base + i:base + i + 1]

        def gfin(i):
            return GAINS[:, DEPTH * 26 + i:DEPTH * 26 + i + 1]

        def ARp(i):
            return ("AR", i)
        QT0, KT0, VT0, SG0, YR0, QX0 = 0, 4, 8, 12, 16, 20

        op("sp", "dma_start", out=CST[:], in_=cst_d[:, :], w=[("CST", 0)], dma=csem)
        last_s = op("sp", "dma_start", out=GAINS[:], in_=gains_d[:, :], w=[("GAINS", 0)], dma=csem)
        sc.mark_written([("CST", 0), ("GAINS", 0)], last_s)
        op("pool", "dma_start", out=CB[:], in_=cstb_d[:, :], w=[("CB", 0)], dma=csem2)
        last_c = op("pool", "dma_start", out=WMIX[:], in_=wmix_d[:, :], w=[("WMIX", 0)], dma=csem2)
        sc.mark_written([("CB", 0), ("WMIX", 0)], last_c)
        for l in range(DEPTH):
            lastop = None
            for name, wd in SLABS:
                off = SLAB_OFF[name][0]
                lastop = op("pool", "dma_start", out=wb_d[l, :, off:off + wd], in_=wf_d[l, :, off:off + wd],
                            dma=castsem[l])
            sc.mark_written([("WB", l)], lastop)
        op("pool", "memset", R32[:], 0.0, w=[("R32", l) for l in range(DEPTH)])
        op("pool", "memset", RBF[:], 0.0, w=[("RBF", l) for l in range(DEPTH)])
        op("pool", "memset", HALO[:], 0.0, w=[("HALO", l) for l in range(DEPTH)])
        op("pool", "memset", MV[:], 0.0, w=[("MV", l) for l in range(DEPTH)])

        stage('loads')
        slab_list = [(l, "memkv") for l in range(DEPTH)]
        for t in range(NT):
            for l in range(DEPTH):
                for name, _ in SLABS[1:]:
                    slab_list.append((l, name))
        ring = {"cur": 0}

        def ring_issue(i):
            if i >= len(slab_list) or stopped[0]:
                return
            l, name = slab_list[i]
            off, wd = SLAB_OFF[name]
            b = i % NB
            op("sp", "dma_start", out=WR[:, b, 0:wd], in_=wb_d[l, :, off:off + wd],
               r=[("WB", l)], w=[("W", b)], dma=wsem[b])

        def ring_next(l, name):
            i = ring["cur"]
            if stopped[0]:
                return i, 0
            assert slab_list[i] == (l, name), (slab_list[i], l, name)
            ring["cur"] = i + 1
            return i, i % NB

        def ring_done(i):
            ring_issue(i + NB)

        for i in range(NB):
            ring_issue(i)

        def rms_stats(src_ap_fn, src_parts, ncols, scale, dst):
            b = nbank()
            for kc in range(KC):
                s = kc % 2
                op("act", "activation", out=SQ[:, s, 0:ncols], in_=src_ap_fn(kc), func=AF.Square,
                   r=[src_parts(kc)], w=[("SQ", s)])
                op("pe", "matmul", PS[:, b, 0:ncols], lhsT=ones, rhs=SQ[:, s, 0:ncols],
                   start=(kc == 0), stop=(kc == KC - 1), r=[("SQ", s), ("CB", 0)], w=[PSp(b)])
            op("act", "activation", out=TT[:, 0:ncols], in_=PS[:, b, 0:ncols], func=AF.Ln,
               scale=scale, bias=cs(C_EPS), r=[PSp(b), ("CST", 0)], w=[("TT", 0)])
            op("act", "activation", out=dst[:, 0:ncols], in_=TT[:, 0:ncols], func=AF.Exp, scale=-0.5,
               r=[("TT", 0)], w=[("RSTD", 0)])

        op("sp", "dma_start", out=X[:, :, 0:MEM], in_=memT_d.rearrange("(kc p) m -> p kc m", p=128),
           w=[("X", kc) for kc in range(KC)], dma=xsem)
        rms_stats(lambda kc: X[:, kc, 0:MEM], lambda kc: ("X", kc), MEM, 1.0 / D, RSTD)
        for l in range(DEPTH):
            for kc in range(KC):
                op("dve", "scalar_tensor_tensor", out=Hh[:, kc, 0:MEM], in0=X[:, kc, 0:MEM],
                   scalar=gcol(l, "mem", kc), in1=RSTD[:, 0:MEM], op0=ALU.mult, op1=ALU.mult,
                   r=[("X", kc), ("RSTD", 0), ("GAINS", 0)], w=[("H", kc)])
            wi, wbuf = ring_next(l, "memkv")
            Wv = WR[:, wbuf, 0:4096].rearrange("p (k n) -> p k n", k=KC)
            for hc in range(2):
                b = nbank()
                for kc in range(KC):
                    op("pe", "matmul", PS[:, b, 0:MEM], lhsT=Wv[:, kc, hc * 128:(hc + 1) * 128],
                       rhs=Hh[:, kc, 0:MEM], start=(kc == 0), stop=(kc == KC - 1),
                       r=[("W", wbuf), ("H", kc)], w=[PSp(b)])
                op("act", "activation", out=MK[:, l, hc, :], in_=PS[:, b, 0:MEM], func=AF.Copy,
                   r=[PSp(b)], w=[("MK", l)])
            for mc in range(2):
                b = nbank()
                for kc in range(KC):
                    op("pe", "matmul", PS[:, b, 0:256], lhsT=Hh[:, kc, mc * 128:(mc + 1) * 128],
                       rhs=Wv[:, kc, 256:512], start=(kc == 0), stop=(kc == KC - 1),
                       r=[("W", wbuf), ("H", kc)], w=[PSp(b)])
                for h in range(H):
                    c0 = (h % 2) * 64
                    op("dve", "tensor_copy", out=MV[:, l, mc, h, c0:c0 + 64], in_=PS[:, b, h * 64:(h + 1) * 64],
                       r=[PSp(b)], w=[("MV", l)])
            ring_done(wi)

        stage('memkv')
        def rope_tables(t):
            op("sp", "dma_start", out=POSI[:], in_=pos_d[0:1, t * T:(t + 1) * T].partition_broadcast(128),
               w=[("POSI", 0)], dma=possem)
            TWO_PI = 6.283185307179586
            C1 = 6.28125
            C2 = TWO_PI - C1
            PI = 3.141592653589793
            PIS = 3.1415925
            op("dve", "tensor_copy", out=RA[:], in_=POSI[:], r=[("POSI", 0)], w=[("RA", 0)])
            op("dve", "tensor_scalar", out=RB[:], in0=RA[:], scalar1=cs(C_INVF), scalar2=None, op0=ALU.mult,
               r=[("RA", 0), ("CST", 0)], w=[("RB", 0)])
            op("dve", "tensor_scalar", out=POSI[:], in0=RB[:], scalar1=1.0 / TWO_PI, scalar2=None, op0=ALU.mult,
               r=[("RB", 0)], w=[("POSI", 0)])
            op("dve", "tensor_copy", out=RA[:], in_=POSI[:], r=[("POSI", 0)], w=[("RA", 0)])
            op("dve", "scalar_tensor_tensor", out=RB[:], in0=RA[:], scalar=-C1, in1=RB[:], op0=ALU.mult, op1=ALU.add,
               r=[("RA", 0), ("RB", 0)], w=[("RB", 0)])
            op("dve", "scalar_tensor_tensor", out=RB[:], in0=RA[:], scalar=-C2, in1=RB[:], op0=ALU.mult, op1=ALU.add,
               r=[("RA", 0), ("RB", 0)], w=[("RB", 0)])
            op("dve", "tensor_scalar", out=RA[:], in0=RB[:], scalar1=-PIS, scalar2=PIS, op0=ALU.max, op1=ALU.min,
               r=[("RB", 0)], w=[("RA", 0)])
            op("act", "activation", out=SIN[:], in_=RA[:], func=AF.Sin, scale=cs(C_SGN),
               r=[("RA", 0), ("CST", 0)], w=[("SIN", 0)])
            op("dve", "tensor_scalar", out=TT[:], in0=RB[:], scalar1=PI / 2, scalar2=None, op0=ALU.add,
               r=[("RB", 0)], w=[("TT", 0)])
            op("dve", "tensor_scalar", out=HR[:], in0=TT[:], scalar1=PI, scalar2=-TWO_PI, op0=ALU.is_gt, op1=ALU.mult,
               r=[("TT", 0)], w=[("HR", 0)])
            op("dve", "tensor_tensor", out=TT[:], in0=TT[:], in1=HR[:], op=ALU.add,
               r=[("TT", 0), ("HR", 0)], w=[("TT", 0)])
            op("dve", "tensor_scalar", out=TT[:], in0=TT[:], scalar1=-PIS, scalar2=PIS, op0=ALU.max, op1=ALU.min,
               r=[("TT", 0)], w=[("TT", 0)])
            op("act", "activation", out=COS[:], in_=TT[:], func=AF.Sin, r=[("TT", 0)], w=[("COS", 0)])

        stats = {"bank": None}

        def stats_acc(kc):
            if kc == 0:
                b = nbank()
                reserved.add(b)
                stats["bank"] = b
            b = stats["bank"]
            sq = kc % 2
            op("act", "activation", out=SQ[:, sq, :], in_=X[:, kc, :], func=AF.Square,
               r=[("X", kc)], w=[("SQ", sq)])
            op("pe", "matmul", PS[:, b, :], lhsT=ones, rhs=SQ[:, sq, :], start=(kc == 0), stop=(kc == KC - 1),
               r=[("SQ", sq), ("CB", 0)], w=[PSp(b)])

        def stats_finish():
            b = stats["bank"]
            assert b is not None
            op("act", "activation", out=TT[:], in_=PS[:, b, :], func=AF.Ln, scale=1.0 / D, bias=cs(C_EPS),
               r=[PSp(b), ("CST", 0)], w=[("TT", 0)])
            op("act", "activation", out=RSTD[:], in_=TT[:], func=AF.Exp, scale=-0.5, r=[("TT", 0)], w=[("RSTD", 0)])
            reserved.discard(b)
            stats["bank"] = None

        def norm_to_h(l, kind):
            stats_finish()
            for kc in range(KC):
                op("dve", "scalar_tensor_tensor", out=Hh[:, kc, :], in0=X[:, kc, :], scalar=gcol(l, kind, kc),
                   in1=RSTD[:], op0=ALU.mult, op1=ALU.mult,
                   r=[("X", kc), ("RSTD", 0), ("GAINS", 0)], w=[("H", kc)])

        def proj_chunk(Wv, wbuf, j, b):
            for kc in range(KC):
                op("pe", "matmul", PS[:, b, :], lhsT=Wv[:, kc, j * 128:(j + 1) * 128], rhs=Hh[:, kc, :],
                   start=(kc == 0), stop=(kc == KC - 1), r=[("W", wbuf), ("H", kc)], w=[PSp(b)])

        def rope_chunk(b, dst_part_idx, qbi):
            op("act", "activation", out=QB[:, qbi, :], in_=PS[:, b, :], func=AF.Copy, r=[PSp(b)], w=[("QB", qbi)])
            b2 = nbank()
            op("pe", "matmul", PS[:, b2, :], lhsT=perm, rhs=QB[:, qbi, :], start=True, stop=True,
               r=[("QB", qbi), ("CB", 0)], w=[PSp(b2)])
            op("dve", "tensor_tensor", out=RA[:], in0=PS[:, b, :], in1=COS[:], op=ALU.mult,
               r=[PSp(b), ("COS", 0)], w=[("RA", 0)])
            op("dve", "tensor_tensor", out=RB[:], in0=PS[:, b2, :], in1=SIN[:], op=ALU.mult,
               r=[PSp(b2), ("SIN", 0)], w=[("RB", 0)])
            op("pool", "tensor_tensor", out=AR[:, dst_part_idx, :], in0=RA[:], in1=RB[:], op=ALU.add,
               r=[("RA", 0), ("RB", 0)], w=[ARp(dst_part_idx)])

        def mixer(t, l):
            norm_to_h(l, "mix")
            stage('norm')
            wi, wbuf = ring_next(l, "ux")
            Wv = WR[:, wbuf, 0:4096].rearrange("p (k n) -> p k n", k=KC)
            op("pool", "tensor_copy", out=U[:, :, 0:15], in_=HALO[:, l, :, :], r=[("HALO", l)], w=[("U", 0)])
            for c in range(2):
                b = nbank()
                proj_chunk(Wv, wbuf, c, b)
                op("dve", "tensor_copy", out=U[:, c, 15:15 + T], in_=PS[:, b, :], r=[PSp(b)], w=[("U", 0)])
            for c in range(2):
                b = nbank()
                proj_chunk(Wv, wbuf, 2 + c, b)
                op("act", "activation", out=AR[:, QX0 + c, :], in_=PS[:, b, :], func=AF.Copy,
                   r=[PSp(b)], w=[ARp(QX0 + c)])
            ring_done(wi)

            op("pool", "tensor_tensor", out=PB[:, :, 1:T + 15], in0=U[:, :, 1:T + 15], in1=U[:, :, 0:T + 14], op=ALU.add,
               r=[("U", 0)], w=[("PB", 0)])
            op("pool", "tensor_tensor", out=PC[64:128, 0, 3:T + 15], in0=PB[64:128, 0, 3:T + 15], in1=PB[64:128, 0, 1:T + 13],
               op=ALU.add, r=[("PB", 0)], w=[("PC", 0)])
            op("pool", "tensor_tensor", out=PC[:, 1, 3:T + 15], in0=PB[:, 1, 3:T + 15], in1=PB[:, 1, 1:T + 13],
               op=ALU.add, r=[("PB", 0)], w=[("PC", 0)])
            op("pool", "tensor_tensor", out=PB[:, 1, 7:T + 15], in0=PC[:, 1, 7:T + 15], in1=PC[:, 1, 3:T + 11],
               op=ALU.add, r=[("PC", 0)], w=[("PB", 0)])
            op("pool", "tensor_tensor", out=PC[64:128, 1, 15:T + 15], in0=PB[64:128, 1, 15:T + 15], in1=PB[64:128, 1, 7:T + 7],
               op=ALU.add, r=[("PB", 0)], w=[("PC", 0)])
            wins = [(PB, 0, 0), (PC, 0, 64), (PB, 1, 0), (PC, 1, 64)]
            for g, (buf, c, p0) in enumerate(wins):
                bn_ = "PB" if buf is PB else "PC"
                if t <= 1:
                    op("pool", "tensor_tensor", out=buf[p0:p0 + 64, c, 15:31], in0=buf[p0:p0 + 64, c, 15:31],
                       in1=CST[p0:p0 + 64, C_CORR + t * 32 + c * 16:C_CORR + t * 32 + c * 16 + 16], op=ALU.mult,
                       r=[(bn_, 0), ("CST", 0)], w=[(bn_, 0)])
                op("dve", "scalar_tensor_tensor", out=POOLED[p0:p0 + 64, c, :], in0=buf[p0:p0 + 64, c, 15:T + 15],
                   scalar=CST[p0:p0 + 64, C_INVW + c:C_INVW + c + 1], in1=U[p0:p0 + 64, c, 15:T + 15],
                   op0=ALU.mult, op1=ALU.subtract, r=[(bn_, 0), ("U", 0), ("CST", 0)], w=[("POOLED", c)])
            op("pool", "tensor_copy", out=HALO[:, l, :, :], in_=U[:, :, T:T + 15], r=[("U", 0)], w=[("HALO", l)])
            wi, wbuf = ring_next(l, "q")
            Wv = WR[:, wbuf, 0:4096].rearrange("p (k n) -> p k n", k=KC)
            for h in range(H):
                b = nbank()
                proj_chunk(Wv, wbuf, h, b)
                rope_chunk(b, QT0 + h, h % 2)
            ring_done(wi)
            stage('q')
            wi, wbuf = ring_next(l, "k")
            Wv = WR[:, wbuf, 0:4096].rearrange("p (k n) -> p k n", k=KC)
            for h in range(H):
                b = nbank()
                proj_chunk(Wv, wbuf, h, b)
                rope_chunk(b, KT0 + h, h % 2)
            ring_done(wi)
            stage('k')
            wi, wbuf = ring_next(l, "v")
            Wv = WR[:, wbuf, 0:4096].rearrange("p (k n) -> p k n", k=KC)
            for c in range(NCH):
                b = nbank()
                for kc in range(KC):
                    op("pe", "matmul", PS[:, b, :], lhsT=Hh[:, kc, c * 128:(c + 1) * 128], rhs=Wv[:, kc, :],
                       start=(kc == 0), stop=(kc == KC - 1), r=[("W", wbuf), ("H", kc)], w=[PSp(b)])
                op("act", "activation", out=AR[:, VT0 + c, :], in_=PS[:, b, :], func=AF.Copy,
                   r=[PSp(b)], w=[ARp(VT0 + c)])
            ring_done(wi)
            stage('v')
            wi, wbuf = ring_next(l, "gr")
            Wv = WR[:, wbuf, 0:4096].rearrange("p (k n) -> p k n", k=KC)
            for h in range(H):
                b = nbank()
                proj_chunk(Wv, wbuf, h, b)
                op("act", "activation", out=AR[:, SG0 + h, :], in_=PS[:, b, :], func=AF.Silu,
                   r=[PSp(b)], w=[ARp(SG0 + h)])
            ring_done(wi)
            stage('gr')
            stage('ux')
            maskT = CST[:, C_MASK:C_MASK + 512]
            ksc = CST[:, C_KSC:C_KSC + 512]
            gct = CST[:, C_GC:C_GC + 512]
            epsr = CST[:, C_EPSR:C_EPSR + 512]
            for c in range(NCH):
                cl = slice(c * 128, (c + 1) * 128)
                bs = nbank()
                for h in range(H):
                    op("pe", "matmul", PS[:, bs, h * 128:(h + 1) * 128], lhsT=AR[:, KT0 + h, cl], rhs=AR[:, QT0 + h, cl],
                       start=True, stop=True, r=[ARp(KT0 + h), ARp(QT0 + h)], w=[PSp(bs)])
                si = c % 2
                op("dve", "tensor_tensor", out=ST[:, si, :], in0=PS[:, bs, :], in1=maskT, op=ALU.mult,
                   r=[PSp(bs), ("CST", 0)], w=[("ST", si)])
                pb = nptb()
                for h in range(H):
                    op("pe", "transpose", PT[:, pb, h * 128:(h + 1) * 128], AR[:, KT0 + h, cl], ident,
                       r=[ARp(KT0 + h), ("CB", 0)], w=[("PT", pb)])
                op("dve", "tensor_tensor", out=KTOK[:, si, :], in0=PT[:, pb, :], in1=ksc, op=ALU.mult,
                   r=[("PT", pb), ("CST", 0)], w=[("KTOK", si)])
                bo = nbank()
                for h in range(H):
                    hs = slice(h * 128, (h + 1) * 128)
                    op("pe", "matmul", PS[:, bo, hs], lhsT=AR[:, VT0 + c, hs], rhs=ST[:, si, hs],
                       start=True, stop=False, r=[ARp(VT0 + c), ("ST", si)], w=[PSp(bo)])
                    op("pe", "matmul", PS[:, bo, hs], lhsT=RBF[:, l, hs], rhs=AR[:, QT0 + h, cl],
                       start=False, stop=True, r=[("RBF", l), ARp(QT0 + h)], w=[PSp(bo)])
                bk = nbank()
                for h in range(H):
                    hs = slice(h * 128, (h + 1) * 128)
                    op("pe", "matmul", PS[:, bk, hs], lhsT=KTOK[:, si, hs], rhs=AR[:, VT0 + c, hs],
                       start=True, stop=True, r=[("KTOK", si), ARp(VT0 + c)], w=[PSp(bk)])
                op("dve", "tensor_tensor", out=Y1[:], in0=R32[:, l, :], in1=PS[:, bk, :], op=ALU.add,
                   r=[("R32", l), PSp(bk)], w=[("Y1", 0)])
                op("pool", "tensor_tensor", out=RBF[:, l, :], in0=Y1[:], in1=gct, op=ALU.mult,
                   r=[("Y1", 0), ("CST", 0)], w=[("RBF", l)])
                op("dve", "tensor_tensor", out=R32[:, l, :], in0=Y1[:], in1=gct, op=ALU.mult,
                   r=[("Y1", 0), ("CST", 0)], w=[("R32", l)])
                op("act", "activation", out=YSQ[:, si, :], in_=PS[:, bo, :], func=AF.Square,
                   r=[PSp(bo)], w=[("YSQ", si)])
                bn = nbank()
                op("pe", "matmul", PS[:, bn, :], lhsT=ones, rhs=YSQ[:, si, :], start=True, stop=True,
                   r=[("YSQ", si), ("CB", 0)], w=[PSp(bn)])
                op("dve", "scalar_tensor_tensor", out=TT[:], in0=PS[:, bn, :], scalar=1.0 / 128, in1=epsr,
                   op0=ALU.mult, op1=ALU.add, r=[PSp(bn), ("CST", 0)], w=[("TT", 0)])
                op("act", "activation", out=TT[:], in_=TT[:], func=AF.Ln, r=[("TT", 0)], w=[("TT", 0)])
                op("act", "activation", out=HR[:], in_=TT[:], func=AF.Exp, scale=-0.5, r=[("TT", 0)], w=[("HR", 0)])
                op("dve", "tensor_tensor", out=RA[:], in0=PS[:, bo, :], in1=HR[:], op=ALU.mult,
                   r=[PSp(bo), ("HR", 0)], w=[("RA", 0)])
                op("pool", "tensor_tensor", out=AR[:, YR0:YR0 + 4, cl],
                   in0=RA[:].rearrange("p (h i) -> p h i", h=H), in1=AR[:, SG0:SG0 + 4, cl], op=ALU.mult,
                   r=[("RA", 0)] + [ARp(SG0 + h) for h in range(H)], w=[ARp(YR0 + h) for h in range(H)])

            stage('ret')
            for c in range(2):
                b = nbank()
                op("pe", "matmul", PS[:, b, :], lhsT=WMIX[:, (l * 2 + c) * 128:(l * 2 + c + 1) * 128], rhs=POOLED[:, c, :],
                   start=True, stop=True, r=[("WMIX", 0), ("POOLED", c)], w=[PSp(b)])
                op("act", "activation", out=YPOOL[:, c, :], in_=PS[:, b, :], func=AF.Copy, scale=gcol(l, "ps", c),
                   r=[PSp(b), ("GAINS", 0)], w=[("YPOOL", c)])

            stage('pool')
            for hc in range(2):
                ba = nbank()
                bd = nbank()
                for hh in range(2):
                    h = 2 * hc + hh
                    r0 = hh * 64
                    eb = h % 2
                    bx = [nbank(), nbank()]
                    for mc in range(2):
                        op("pe", "matmul", PS[:, bx[mc], :], lhsT=MK[r0:r0 + 64, l, hc, mc * 128:(mc + 1) * 128],
                           rhs=AR[r0:r0 + 64, QX0 + hc, :], start=True, stop=True,
                           r=[("MK", l), ARp(QX0 + hc)], w=[PSp(bx[mc])])
                    for mc in range(2):
                        op("act", "activation", out=EXPT[:, eb, mc, :], in_=PS[:, bx[mc], :], func=AF.Exp, scale=0.125,
                           r=[PSp(bx[mc])], w=[("EXPT", eb)])
                    for mc in range(2):
                        first = (hh == 0 and mc == 0)
                        last = (hh == 1 and mc == 1)
                        op("pe", "matmul", PS[:, ba, :], lhsT=MV[:, l, mc, h, :], rhs=EXPT[:, eb, mc, :],
                           start=first, stop=last, r=[("MV", l), ("EXPT", eb)], w=[PSp(ba)])
                        op("pe", "matmul", PS[:, bd, :], lhsT=CB[:, B_OPAD + hh * 128:B_OPAD + (hh + 1) * 128],
                           rhs=EXPT[:, eb, mc, :], start=first, stop=last, r=[("CB", 0), ("EXPT", eb)], w=[PSp(bd)])
                op("dve", "reciprocal", out=RDEN[:], in_=PS[:, bd, :], r=[PSp(bd)], w=[("RDEN", 0)])
                op("dve", "tensor_tensor", out=YX[:, hc, :], in0=PS[:, ba, :], in1=RDEN[:], op=ALU.mult,
                   r=[PSp(ba), ("RDEN", 0)], w=[("YX", hc)])

            stage('xattn')
            for c in range(8):
                wi, wbuf = ring_next(l, f"mg{c}")
                Wg = WR[:, wbuf, 0:3072].rearrange("p (k n) -> p k n", k=KC)
                Wr = WR[:, wbuf, 3072:3584].rearrange("p (k n) -> p k n", k=4)
                Wp = WR[:, wbuf, 3584:3840].rearrange("p (k n) -> p k n", k=2)
                Wx = WR[:, wbuf, 3840:4096].rearrange("p (k n) -> p k n", k=2)
                ts = (c % 2) * 3
                bg = []
                for j in range(3):
                    b = nbank()
                    bg.append(b)
                    proj_chunk(Wg, wbuf, j, b)
                    op("act", "activation", out=TG[:, ts + j, :], in_=PS[:, b, :], func=AF.Tanh, scale=0.5,
                       r=[PSp(b)], w=[("TG", ts + j)])
                br = nbank()
                for h in range(H):
                    op("pe", "matmul", PS[:, br, :], lhsT=Wr[:, h, :], rhs=AR[:, YR0 + h, :], start=(h == 0), stop=(h == H - 1),
                       r=[("W", wbuf), ARp(YR0 + h)], w=[PSp(br)])
                bp = nbank()
                for j in range(2):
                    op("pe", "matmul", PS[:, bp, :], lhsT=Wp[:, j, :], rhs=YPOOL[:, j, :], start=(j == 0), stop=(j == 1),
                       r=[("W", wbuf), ("YPOOL", j)], w=[PSp(bp)])
                bxx = nbank()
                for j in range(2):
                    op("pe", "matmul", PS[:, bxx, :], lhsT=Wx[:, j, :], rhs=YX[:, j, :], start=(j == 0), stop=(j == 1),
                       r=[("W", wbuf), ("YX", j)], w=[PSp(bxx)])
                ring_done(wi)
                for j, bb in enumerate((br, bp, bxx)):
                    op("dve", "scalar_tensor_tensor", out=Mm[:, ts + j, :], in0=TG[:, ts + j, :], scalar=1.0, in1=PS[:, bb, :],
                       op0=ALU.add, op1=ALU.mult, r=[("TG", ts + j), PSp(bb)], w=[("Mm", ts + j)])
                op("pool", "tensor_tensor", out=Mm[:, ts, :], in0=Mm[:, ts, :], in1=Mm[:, ts + 1, :], op=ALU.add,
                   r=[("Mm", ts), ("Mm", ts + 1)], w=[("Mm", ts)])
                op("pool", "tensor_tensor", out=AR[:, c, :], in0=Mm[:, ts, :], in1=Mm[:, ts + 2, :], op=ALU.add,
                   r=[("Mm", ts), ("Mm", ts + 2)], w=[ARp(c)])

            stage('merge')
            for s in range(2):
                wi, wbuf = ring_next(l, f"wo{s}")
                Wv = WR[:, wbuf, 0:4096].rearrange("p (k n) -> p k n", k=KC)
                for j in range(4):
                    oc = s * 4 + j
                    b = nbank()
                    for kc in range(KC):
                        op("pe", "matmul", PS[:, b, :], lhsT=Wv[:, kc, j * 128:(j + 1) * 128], rhs=AR[:, kc, :],
                           start=(kc == 0), stop=(kc == KC - 1), r=[("W", wbuf), ARp(kc)], w=[PSp(b)])
                    op("dve", "scalar_tensor_tensor", out=X[:, oc, :], in0=PS[:, b, :], scalar=0.5, in1=X[:, oc, :],
                       op0=ALU.mult, op1=ALU.add, r=[PSp(b), ("X", oc)], w=[("X", oc)])
                    if oc >= 1:
                        stats_acc(oc - 1)
                ring_done(wi)
            stats_acc(KC - 1)

        def ffn(t, l):
            norm_to_h(l, "ffn")
            for s in range(11):
                wi, wbuf = ring_next(l, f"fi{s}")
                Wv = WR[:, wbuf, 0:4096].rearrange("p (k n) -> p k n", k=KC)
                for j in range(2):
                    c = 2 * s + j
                    ba = nbank()
                    proj_chunk(Wv, wbuf, j, ba)
                    bb = nbank()
                    proj_chunk(Wv, wbuf, 2 + j, bb)
                    ti = c % 2
                    op("act", "activation", out=TH[:, ti, :], in_=PS[:, ba, :], func=AF.Silu,
                       r=[PSp(ba)], w=[("TH", ti)])
                    op("dve", "tensor_tensor", out=AR[:, c, :], in0=TH[:, ti, :], in1=PS[:, bb, :], op=ALU.mult,
                       r=[("TH", ti), PSp(bb)], w=[ARp(c)])
                ring_done(wi)
            for oc in range(8):
                wi, wbuf = ring_next(l, f"fo{oc}")
                Wv = WR[:, wbuf, 0:2816].rearrange("p (k n) -> p k n", k=FKC)
                b = nbank()
                for kc in range(FKC):
                    op("pe", "matmul", PS[:, b, :], lhsT=Wv[:, kc, :], rhs=AR[:, kc, :],
                       start=(kc == 0), stop=(kc == FKC - 1), r=[("W", wbuf), ARp(kc)], w=[PSp(b)])
                op("dve", "tensor_tensor", out=X[:, oc, :], in0=PS[:, b, :], in1=X[:, oc, :], op=ALU.add,
                   r=[PSp(b), ("X", oc)], w=[("X", oc)])
                if oc >= 1:
                    stats_acc(oc - 1)
                ring_done(wi)
            stats_acc(KC - 1)

        xT_v = xT_d.rearrange("(kc p) s -> p kc s", p=128)
        out_v = out_d.rearrange("(kc p) s -> p kc s", p=128)
        last_store = None
        groups = [[2 * i, 2 * i + 1] for i in range(npair)]
        Xparts = [("X", kc) for kc in range(KC)]
        for t in range(NT):
            op("sp", "dma_start", out=X[:], in_=xT_v[:, :, t * T:(t + 1) * T], w=Xparts, dma=xsem)
            if t >= 1:
                for kc in range(KC):
                    si = kc % 2
                    op("sp", "dma_start", out=STG[:, si, :], in_=rcv_d[0:128, kc * T:(kc + 1) * T],
                       r=[("RCV", 0)], w=[("STG", si)], dma=stgsem[si])
                    op("dve", "scalar_tensor_tensor", out=X[:, kc, :], in0=STG[:, si, :], scalar=cs(C_SG),
                       in1=X[:, kc, :], op0=ALU.mult, op1=ALU.add,
                       r=[("STG", si), ("X", kc), ("CST", 0)], w=[("X", kc)])
            for kc in range(KC):
                stats_acc(kc)
            rope_tables(t)
            for l in range(DEPTH):
                mixer(t, l)
                ffn(t, l)
            if t == 0:
                op("pool", "tensor_scalar", out=R32[:], in0=R32[:], scalar1=cs(C_KEEP), scalar2=None, op0=ALU.mult,
                   r=[("R32", l) for l in range(DEPTH)] + [("CST", 0)], w=[("R32", l) for l in range(DEPTH)])
                op("pool", "tensor_scalar", out=RBF[:], in0=RBF[:], scalar1=cs(C_KEEP), scalar2=None, op0=ALU.mult,
                   r=[("RBF", l) for l in range(DEPTH)] + [("CST", 0)], w=[("RBF", l) for l in range(DEPTH)])
                op("pool", "tensor_scalar", out=HALO[:], in0=HALO[:], scalar1=cs(C_KEEP), scalar2=None, op0=ALU.mult,
                   r=[("HALO", l) for l in range(DEPTH)] + [("CST", 0)], w=[("HALO", l) for l in range(DEPTH)])
            if t < NT - 1:
                op("sp", "dma_start", out=snd_d.rearrange("p (k n) -> p k n", k=KC), in_=X[:],
                   r=Xparts, w=[("SND", 0)], dma=sndsem)
                op("pool", "collective_compute", "AllGather", ALU.bypass, replica_groups=groups,
                   ins=[snd_d], outs=[rcv_d], r=[("SND", 0)], w=[("RCV", 0)], dma=ccsem, inc=1)
            stats_finish()
            for kc in range(KC):
                op("dve", "scalar_tensor_tensor", out=X[:, kc, :], in0=X[:, kc, :], scalar=gfin(kc),
                   in1=RSTD[:], op0=ALU.mult, op1=ALU.mult,
                   r=[("X", kc), ("RSTD", 0), ("GAINS", 0)], w=[("X", kc)])
            last_store = op("sp", "dma_start", out=out_v[:, :, t * T:(t + 1) * T], in_=X[:], r=Xparts, dma=osem)
        if stopped[0]:
            stopped[0] = False
            last_store = op("sp", "dma_start", out=out_v[:, :, 0:T], in_=X[:],
                            r=[("X", kc) for kc in range(KC)] + [("RSTD", 0)], dma=osem)
        else:
            assert ring["cur"] == len(slab_list)

        block = es.enter_context(nc.Block())

        @block.sync
        def _(e):
            sc.emit("sp", e, esem)
            e.wait_ge(osem, last_store.val)

        @block.tensor
        def _(e):
            sc.emit("pe", e, esem)

        @block.scalar
        def _(e):
            sc.emit("act", e, esem)

        @block.vector
        def _(e):
            sc.emit("dve", e, esem)

        @block.gpsimd
        def _(e):
            sc.emit("pool", e, esem)

    return nc


def _slab(Wm, cols):
    K = Wm.shape[0]
    sub = Wm[:, cols]
    return np.ascontiguousarray(sub.reshape(K // 128, 128, -1).transpose(1, 0, 2)).reshape(128, -1)


def _layer_line(l, w_in, w_up_ret, w_up_pool, w_up_x, w_out, w_ffn_in, w_ffn_out, w_mem_kv):
    ar = np.arange
    parts = {}
    parts["memkv"] = _slab(w_mem_kv[l], ar(512))
    parts["q"] = _slab(w_in[l], ar(0, 512))
    parts["k"] = _slab(w_in[l], ar(512, 1024))
    parts["v"] = _slab(w_in[l], ar(1024, 1536))
    parts["gr"] = _slab(w_in[l], ar(1536, 2048))
    parts["ux"] = _slab(w_in[l], ar(2048, 2560))
    for c in range(8):
        gcols = np.concatenate([2560 + j * 1024 + c * 128 + ar(128) for j in range(3)])
        oc = c * 128 + ar(128)
        parts[f"mg{c}"] = np.concatenate(
            [_slab(w_in[l], gcols), _slab(w_up_ret[l], oc), _slab(w_up_pool[l], oc), _slab(w_up_x[l], oc)], axis=1)
    for s in range(2):
        parts[f"wo{s}"] = _slab(w_out[l], ar(s * 512, (s + 1) * 512))
    for s in range(11):
        cols = np.concatenate([(2 * s) * 128 + ar(128), (2 * s + 1) * 128 + ar(128),
                               FH + (2 * s) * 128 + ar(128), FH + (2 * s + 1) * 128 + ar(128)])
        parts[f"fi{s}"] = _slab(w_ffn_in[l], cols)
    for j in range(8):
        parts[f"fo{j}"] = _slab(w_ffn_out[l], ar(j * 128, (j + 1) * 128))
    line = np.concatenate([parts[n] for n, _ in SLABS], axis=1)
    assert line.shape == (128, LINE), line.shape
    return line


def _consts(stage):
    c = np.zeros((128, NCONST), np.float64)
    g = 1.0 - np.exp2(-5.0 - np.arange(H))
    j = np.arange(128)
    dk = 128.0 ** -0.5
    for h in range(H):
        ginv = g[h] ** (-(j + 1.0))
        m = (j[:, None] <= j[None, :]) * (ginv[:, None] * dk)
        c[:, C_MASK + h * 128:C_MASK + (h + 1) * 128] = m
        c[:, C_KSC + h * 128:C_KSC + (h + 1) * 128] = (ginv * dk)[:, None]
        c[:, C_GC + h * 128:C_GC + (h + 1) * 128] = g[h] ** 128.0
        c[:, C_EPSR + h * 128:C_EPSR + (h + 1) * 128] = (EPS * g[h] ** (-2.0 * (j + 1.0)))[None, :]
    wins = (2, 4, 8, 16)
    for ch in range(2):
        for half in range(2):
            w = wins[ch * 2 + half]
            p = slice(half * 64, half * 64 + 64)
            c[p, C_INVW + ch] = 1.0 / w
            tt = np.arange(16)
            real = (w / np.minimum(tt + 1, w))[None, :]
            for st in range(2):
                val = real if st == stage else 1.0
                c[p, C_CORR + st * 32 + ch * 16:C_CORR + st * 32 + ch * 16 + 16] = val
    inv_freq = (10000.0 ** (-np.arange(0, 128, 2, dtype=np.float32) / np.float32(128))).astype(np.float32)
    c[:, C_INVF] = np.concatenate([inv_freq, inv_freq])
    c[0:64, C_SGN] = -1.0
    c[64:128, C_SGN] = 1.0
    c[:, C_EPS] = EPS
    c[:, C_SG] = float(stage)
    c[:, C_KEEP] = 1.0 - float(stage)
    cb = np.zeros((128, NCB), np.float32)
    cb[:, B_ID:B_ID + 128] = np.eye(128)
    pm = np.zeros((128, 128), np.float32)
    mm = np.arange(128)
    pm[(mm + 64) % 128, mm] = 1.0
    cb[:, B_PERM:B_PERM + 128] = pm
    cb[:, B_ONES:B_ONES + 128] = 1.0
    cb[:, B_OPAD + 0:B_OPAD + 64] = 1.0
    cb[:, B_OPAD + 128 + 64:B_OPAD + 256] = 1.0
    return c.astype(np.float32), cb


_PROG_CACHE = {}


def kernel(x, mem, positions, norm_mix, w_in, w_up_ret, w_pool_mix, pool_scale, w_up_pool,
           norm_mem, w_mem_kv, w_up_x, w_out, norm_ffn, w_ffn_in, w_ffn_out, final_norm):
    x = np.asarray(x, np.float32)
    mem = np.asarray(mem, np.float32)
    B, S, _ = x.shape
    DEPTH = int(np.asarray(norm_mix).shape[0])
    f = lambda a: np.asarray(a, np.float32)
    w_in, w_up_ret, w_up_pool, w_up_x, w_out = f(w_in), f(w_up_ret), f(w_up_pool), f(w_up_x), f(w_out)
    w_ffn_in, w_ffn_out, w_mem_kv, w_pool_mix = f(w_ffn_in), f(w_ffn_out), f(w_mem_kv), f(w_pool_mix)
    norm_mix, norm_ffn, norm_mem, pool_scale, final_norm = f(norm_mix), f(norm_ffn), f(norm_mem), f(pool_scale), f(final_norm)

    LD = DEPTH // 2
    NT = S // T
    SP_ = (NT + 1) * T
    lines = [_layer_line(l, w_in, w_up_ret, w_up_pool, w_up_x, w_out, w_ffn_in, w_ffn_out, w_mem_kv)
             for l in range(DEPTH)]
    pos = np.asarray(positions, np.int32).reshape(S)

    def stage_inputs(stage):
        ls = list(range(stage * LD, (stage + 1) * LD))
        wf = np.stack([lines[l] for l in ls], axis=0)
        cst, cstb = _consts(stage)
        gains = np.zeros((128, LD * 26 + 8), np.float32)
        wmix = np.zeros((128, LD, 2, 128), np.float32)
        for i, l in enumerate(ls):
            gains[:, i * 26 + 0:i * 26 + 8] = norm_mix[l].reshape(8, 128).T
            gains[:, i * 26 + 8:i * 26 + 16] = norm_ffn[l].reshape(8, 128).T
            gains[:, i * 26 + 16:i * 26 + 24] = norm_mem[l].reshape(8, 128).T
            gains[:, i * 26 + 24:i * 26 + 26] = pool_scale[l].reshape(2, 128).T
            for g in range(4):
                c, half = g // 2, g % 2
                wmix[half * 64:half * 64 + 64, i, c, half * 64:half * 64 + 64] = w_pool_mix[l, g]
        gains[:, LD * 26:LD * 26 + 8] = final_norm.reshape(8, 128).T
        posp = np.zeros((1, SP_), np.int32)
        posp[0, stage * T:stage * T + S] = pos
        return dict(wf=wf, cst=cst, cstb=cstb, gains=gains, wmix=wmix.reshape(128, -1), pos=posp)

    st_in = [stage_inputs(0), stage_inputs(1)]
    key = (S, LD, B)
    if key not in _PROG_CACHE:
        _PROG_CACHE[key] = build_program(S, LD, B)
    nc = _PROG_CACHE[key]
    in_maps = []
    for b in range(B):
        memT = np.ascontiguousarray(mem[b].T)
        xa = np.zeros((D, SP_), np.float32)
        xa[:, 0:S] = x[b].T
        in_maps.append(dict(st_in[0], xT=xa, memT=memT))
        in_maps.append(dict(st_in[1], xT=np.zeros((D, SP_), np.float32), memT=memT))
    ncores = 2 * B
    res = run_bass_kernel_spmd(nc, in_maps, core_ids=list(range(ncores)))
    out = np.stack([np.ascontiguousarray(res.results[2 * b + 1]["outT"][:, T:T + S].T) for b in range(B)], axis=0)
    return out.astype(np.float32)
```

```python
import contextlib
import numpy as np
import concourse.bass as bass
import concourse.mybir as mybir
from concourse.bass_utils import run_bass_kernel_spmd

F32, BF16, I32 = mybir.dt.float32, mybir.dt.bfloat16, mybir.dt.int32
ALU = mybir.AluOpType
AF = mybir.ActivationFunctionType

D = 1024
KC = 8
T = 512
NCH = T // 128
H = 4
MEM = 256
FH = 2816
FKC = 22
IN_W = 5632
EPS = 1e-6
NB = 5
SLABW = 4096

SLABS = [("memkv", 4096), ("ux", 4096), ("q", 4096), ("k", 4096), ("v", 4096), ("gr", 4096)]
SLABS += [(f"mg{c}", 4096) for c in range(8)]
SLABS += [("wo0", 4096), ("wo1", 4096)]
SLABS += [(f"fi{s}", 4096) for s in range(11)]
SLABS += [(f"fo{j}", 2816) for j in range(8)]
SLAB_OFF = {}
_o = 0
for _n, _w in SLABS:
    SLAB_OFF[_n] = (_o, _w)
    _o += _w
LINE = _o

C_MASK = 0
C_KSC = 512
C_GC = 1024
C_EPSR = 1536
C_INVW = 2048
C_CORR = 2050
C_INVF = 2114
C_SGN = 2115
C_EPS = 2116
C_SG = 2117
C_KEEP = 2118
NCONST = 2120
B_ID = 0
B_PERM = 128
B_ONES = 256
B_OPAD = 384
NCB = 640


class Op:
    __slots__ = ("eng", "fn", "waits", "idx", "needed", "dma", "val", "inc")


class Sched:
    ENGS = ("pe", "act", "dve", "pool", "sp")

    def __init__(self):
        self.streams = {e: [] for e in self.ENGS}
        self.last_w = {}
        self.readers = {}
        self.known = {e: {} for e in self.ENGS}
        self.dma_count = {}

    def add(self, eng, fn, reads=(), writes=(), dma=None, inc=16):
        op = Op()
        op.eng, op.fn, op.needed, op.dma, op.val = eng, fn, False, dma, None
        op.inc = inc
        st = self.streams[eng]
        op.idx = len(st)
        deps = []
        for p in reads:
            w = self.last_w.get(p)
            if w is not None:
                deps.append((w, True))
            if p[0] in ("PS", "PT"):
                rd = self.readers.get(p)
                if rd:
                    for k_, r in rd.items():
                        if k_ != eng:
                            deps.append((r, True))
        for p in writes:
            w = self.last_w.get(p)
            if w is not None:
                deps.append((w, True))
            rd = self.readers.get(p)
            if rd:
                for r in rd.values():
                    deps.append((r, False))
        waits = {}
        kn = self.known[eng]
        for d, hazard_rw in deps:
            if d is op:
                continue
            if d.dma is not None:
                key = ("dma", id(d.dma))
                if kn.get(key, 0) >= d.val:
                    continue
                kn[key] = d.val
                waits[key] = d
                continue
            if d.eng == eng:
                if eng == "pe" or eng == "sp":
                    continue
            key = d.eng
            if kn.get(key, -1) >= d.idx:
                continue
            kn[key] = d.idx
            d.needed = True
            prev = waits.get(key)
            if prev is None or prev.idx < d.idx:
                waits[key] = d
        op.waits = list(waits.values())
        if dma is not None:
            c = self.dma_count.get(id(dma), 0) + 1
            self.dma_count[id(dma)] = c
            op.val = inc * c
        st.append(op)
        for p in reads:
            self.readers.setdefault(p, {})[eng if dma is None else ("dma", id(op))] = op
        for p in writes:
            self.last_w[p] = op
            self.readers[p] = {}
        return op

    def mark_written(self, parts, op):
        for p in parts:
            self.last_w[p] = op
            self.readers[p] = {}

    def emit(self, eng, e, esem):
        if not hasattr(self, "_vals"):
            self._vals = True
            for en in self.ENGS:
                c = 0
                for op in self.streams[en]:
                    if op.dma is None and op.needed:
                        c += 1
                        op.val = c
        for op in self.streams[eng]:
            for d in op.waits:
                if d.dma is not None:
                    e.wait_ge(d.dma, d.val)
                else:
                    e.wait_ge(esem[d.eng], d.val)
            ins = op.fn(e)
            if op.dma is not None:
                ins.then_inc(op.dma, op.inc)
            elif op.needed:
                ins.then_inc(esem[eng], 1)


class _Stop(Exception):
    pass


def build_program(S, DEPTH, npair=4, stop=None):
    NT = S // T + 1
    SP_ = NT * T
    stage_ctr = [0]
    stopped = [False]

    def stage(name):
        stage_ctr[0] += 1
        if stop is not None and stage_ctr[0] >= stop:
            if not stopped[0]:
                print('STOP at stage', stage_ctr[0], name)
            stopped[0] = True
    nc = bass.Bass("TRN2", target_bir_lowering=False)
    xT_d = nc.dram_tensor("xT", [D, SP_], F32, kind="ExternalInput").ap()
    memT_d = nc.dram_tensor("memT", [D, MEM], F32, kind="ExternalInput").ap()
    pos_d = nc.dram_tensor("pos", [1, SP_], I32, kind="ExternalInput").ap()
    wf_d = nc.dram_tensor("wf", [DEPTH, 128, LINE], F32, kind="ExternalInput").ap()
    cst_d = nc.dram_tensor("cst", [128, NCONST], F32, kind="ExternalInput").ap()
    cstb_d = nc.dram_tensor("cstb", [128, NCB], F32, kind="ExternalInput").ap()
    gains_d = nc.dram_tensor("gains", [128, DEPTH * 26 + 8], F32, kind="ExternalInput").ap()
    wmix_d = nc.dram_tensor("wmix", [128, DEPTH * 2 * 128], F32, kind="ExternalInput").ap()
    out_d = nc.dram_tensor("outT", [D, SP_], F32, kind="ExternalOutput").ap()
    snd_d = [nc.dram_tensor(f"snd{i}", [128, 4 * T], F32, kind="Internal").ap() for i in range(2)]
    rcv_d = [nc.dram_tensor(f"rcv{i}", [256, 4 * T], F32, kind="Internal", addr_space="Local").ap() for i in range(2)]
    wb_d = nc.dram_tensor("wb", [DEPTH, 128, LINE], BF16, kind="Internal").ap()

    es = contextlib.ExitStack()
    with es:
        def sb(name, shape, dt):
            return es.enter_context(nc.sbuf_tensor(name, shape, dt))

        def sem(name):
            return es.enter_context(nc.semaphore(name))

        X = sb("X", [128, KC, T], F32)
        Hh = sb("Hh", [128, KC, T], BF16)
        SQ = sb("SQ", [128, 2, T], BF16)
        RSTD = sb("RSTD", [128, T], F32)
        AR = sb("AR", [128, 22, T], BF16)
        RA = sb("RA", [128, T], F32)
        RB = sb("RB", [128, T], F32)
        QB = sb("QB", [128, 2, T], BF16)
        U = sb("U", [128, 2, T + 15], F32)
        PB = sb("PB", [128, 2, T + 15], F32)
        PC = sb("PC", [128, 2, T + 15], F32)
        POOLED = sb("POOLED", [128, 2, T], BF16)
        YPOOL = sb("YPOOL", [128, 2, T], BF16)
        EXPT = sb("EXPT", [128, 2, 2, T], BF16)
        YX = sb("YX", [128, 2, T], BF16)
        RDEN = sb("RDEN", [128, T], F32)
        ST = sb("ST", [128, 2, T], BF16)
        KTOK = sb("KTOK", [128, 2, T], BF16)
        YSQ = sb("YSQ", [128, 2, T], BF16)
        TT = sb("TT", [128, T], F32)
        HR = sb("HR", [128, T], F32)
        Y1 = sb("Y1", [128, T], F32)
        TG = sb("TG", [128, 6, T], F32)
        Mm = sb("Mm", [128, 6, T], F32)
        TH = sb("TH", [128, 2, T], F32)
        R32 = sb("R32", [128, DEPTH, T], F32)
        RBF = sb("RBF", [128, DEPTH, T], BF16)
        HALO = sb("HALO", [128, DEPTH, 2, 15], F32)
        MK = sb("MK", [128, DEPTH, 2, MEM], BF16)
        MV = sb("MV", [128, DEPTH, 2, 4, 128], BF16)
        WR = sb("WR", [128, NB, SLABW], BF16)
        COS = sb("COS", [128, T], F32)
        SIN = sb("SIN", [128, T], F32)
        POSI = sb("POSI", [128, T], I32)
        STG = sb("STG", [128, 4, T], F32)
        CST = sb("CST", [128, NCONST], F32)
        CB = sb("CB", [128, NCB], BF16)
        GAINS = sb("GAINS", [128, DEPTH * 26 + 8], F32)
        WMIX = sb("WMIX", [128, DEPTH * 2 * 128], BF16)
        PS = es.enter_context(nc.psum_tensor("PS", [128, 7, T], F32))
        PT = es.enter_context(nc.psum_tensor("PT", [128, 2, T], BF16))

        esem = {e: sem("s_" + e) for e in ("pe", "act", "dve", "pool")}
        wsem = [sem(f"w{b}") for b in range(NB)]
        castsem = [sem(f"cast{l}") for l in range(DEPTH)]
        xsem = sem("xld")
        possem = sem("posld")
        osem = sem("ost")
        csem = sem("cld")
        csem2 = sem("cld2")
        stgsem = [sem(f"stg{i}") for i in range(4)]
        sndsem = [sem("snd0"), sem("snd1")]
        ccsem = [sem("cc0"), sem("cc1")]

        sc = Sched()
        groups = [[2 * i, 2 * i + 1] for i in range(npair)]

        def op(eng, method, *args, r=(), w=(), dma=None, inc=16, **kw):
            if stopped[0]:
                return None
            return sc.add(eng, lambda e: getattr(e, method)(*args, **kw), r, w, dma, inc)

        bank_ctr = [0]

        reserved = set()

        def nbank():
            while True:
                b = bank_ctr[0]
                bank_ctr[0] = (b + 1) % 7
                if b not in reserved:
                    return b

        ptc = [0]

        def nptb():
            b = ptc[0]
            ptc[0] = 1 - b
            return b

        def PSp(b):
            return ("PS", b)

        def cs(a, n=1):
            return CST[:, a:a + n]

        ident = CB[:, B_ID:B_ID + 128]
        perm = CB[:, B_PERM:B_PERM + 128]
        ones = CB[:, B_ONES:B_ONES + 128]

        def gcol(l, kind, i):
            base = l * 26 + {"mix": 0, "ffn": 8, "mem": 16, "ps": 24}[kind]
            return GAINS[:, base + i:base + i + 1]

        def gfin(i):
            return GAINS[:, DEPTH * 26 + i:DEPTH * 26 + i + 1]

        def ARp(i):
            return ("AR", i)
        QT0, KT0, VT0, SG0, YR0, QX0 = 0, 4, 8, 12, 16, 20

        op("sp", "dma_start", out=CST[:], in_=cst_d[:, :], w=[("CST", 0)], dma=csem)
        last_s = op("sp", "dma_start", out=GAINS[:], in_=gains_d[:, :], w=[("GAINS", 0)], dma=csem)
        sc.mark_written([("CST", 0), ("GAINS", 0)], last_s)
        op("pool", "dma_start", out=CB[:], in_=cstb_d[:, :], w=[("CB", 0)], dma=csem2)
        last_c = op("pool", "dma_start", out=WMIX[:], in_=wmix_d[:, :], w=[("WMIX", 0)], dma=csem2)
        mo, mw = SLAB_OFF["memkv"]
        for l in range(DEPTH):
            last_c = op("pool", "dma_start", out=wb_d[l, :, mo:mo + mw], in_=wf_d[l, :, mo:mo + mw], dma=csem2)
        sc.mark_written([("CB", 0), ("WMIX", 0)] + [("WBM", l) for l in range(DEPTH)], last_c)
        for l in range(DEPTH):
            lastop = None
            for name, wd in SLABS[1:]:
                off = SLAB_OFF[name][0]
                lastop = op("pool", "dma_start", out=wb_d[l, :, off:off + wd], in_=wf_d[l, :, off:off + wd],
                            dma=castsem[l])
            sc.mark_written([("WB", l)], lastop)
        op("pool", "memset", R32[:], 0.0, w=[("R32", l) for l in range(DEPTH)])
        op("pool", "memset", RBF[:], 0.0, w=[("RBF", l) for l in range(DEPTH)])
        op("pool", "memset", HALO[:], 0.0, w=[("HALO", l) for l in range(DEPTH)])
        op("pool", "memset", MV[:], 0.0, w=[("MV", l) for l in range(DEPTH)])

        stage('loads')
        slab_list = [(l, "memkv") for l in range(DEPTH)]
        for t in range(NT):
            for l in range(DEPTH):
                for name, _ in SLABS[1:]:
                    slab_list.append((l, name))
        ring = {"cur": 0}

        def ring_issue(i):
            if i >= len(slab_list) or stopped[0]:
                return
            l, name = slab_list[i]
            off, wd = SLAB_OFF[name]
            b = i % NB
            op("sp", "dma_start", out=WR[:, b, 0:wd], in_=wb_d[l, :, off:off + wd],
               r=[("WBM", l) if name == "memkv" else ("WB", l)], w=[("W", b)], dma=wsem[b])

        def ring_next(l, name):
            i = ring["cur"]
            if stopped[0]:
                return i, 0
            assert slab_list[i] == (l, name), (slab_list[i], l, name)
            ring["cur"] = i + 1
            return i, i % NB

        def ring_done(i):
            ring_issue(i + NB)

        for i in range(NB):
            ring_issue(i)

        def rms_stats(src_ap_fn, src_parts, ncols, scale, dst):
            b = nbank()
            for kc in range(KC):
                s = kc % 2
                op("act", "activation", out=SQ[:, s, 0:ncols], in_=src_ap_fn(kc), func=AF.Square,
                   r=[src_parts(kc)], w=[("SQ", s)])
                op("pe", "matmul", PS[:, b, 0:ncols], lhsT=ones, rhs=SQ[:, s, 0:ncols],
                   start=(kc == 0), stop=(kc == KC - 1), r=[("SQ", s), ("CB", 0)], w=[PSp(b)])
            op("act", "activation", out=TT[:, 0:ncols], in_=PS[:, b, 0:ncols], func=AF.Ln,
               scale=scale, bias=cs(C_EPS), r=[PSp(b), ("CST", 0)], w=[("TT", 0)])
            op("act", "activation", out=dst[:, 0:ncols], in_=TT[:, 0:ncols], func=AF.Exp, scale=-0.5,
               r=[("TT", 0)], w=[("RSTD", 0)])

        op("sp", "dma_start", out=X[:, :, 0:MEM], in_=memT_d.rearrange("(kc p) m -> p kc m", p=128),
           w=[("X", kc) for kc in range(KC)], dma=xsem)
        rms_stats(lambda kc: X[:, kc, 0:MEM], lambda kc: ("X", kc), MEM, 1.0 / D, RSTD)
        for l in range(DEPTH):
            for kc in range(KC):
                op("dve", "scalar_tensor_tensor", out=Hh[:, kc, 0:MEM], in0=X[:, kc, 0:MEM],
                   scalar=gcol(l, "mem", kc), in1=RSTD[:, 0:MEM], op0=ALU.mult, op1=ALU.mult,
                   r=[("X", kc), ("RSTD", 0), ("GAINS", 0)], w=[("H", kc)])
            wi, wbuf = ring_next(l, "memkv")
            Wv = WR[:, wbuf, 0:4096].rearrange("p (k n) -> p k n", k=KC)
            for hc in range(2):
                b = nbank()
                for kc in range(KC):
                    op("pe", "matmul", PS[:, b, 0:MEM], lhsT=Wv[:, kc, hc * 128:(hc + 1) * 128],
                       rhs=Hh[:, kc, 0:MEM], start=(kc == 0), stop=(kc == KC - 1),
                       r=[("W", wbuf), ("H", kc)], w=[PSp(b)])
                op("act", "activation", out=MK[:, l, hc, :], in_=PS[:, b, 0:MEM], func=AF.Copy,
                   r=[PSp(b)], w=[("MK", l)])
            for mc in range(2):
                b = nbank()
                for kc in range(KC):
                    op("pe", "matmul", PS[:, b, 0:256], lhsT=Hh[:, kc, mc * 128:(mc + 1) * 128],
                       rhs=Wv[:, kc, 256:512], start=(kc == 0), stop=(kc == KC - 1),
                       r=[("W", wbuf), ("H", kc)], w=[PSp(b)])
                for h in range(H):
                    c0 = (h % 2) * 64
                    op("dve", "tensor_copy", out=MV[:, l, mc, h, c0:c0 + 64], in_=PS[:, b, h * 64:(h + 1) * 64],
                       r=[PSp(b)], w=[("MV", l)])
            ring_done(wi)

        stage('memkv')
        def rope_tables(t):
            op("sp", "dma_start", out=POSI[:], in_=pos_d[0:1, t * T:(t + 1) * T].partition_broadcast(128),
               w=[("POSI", 0)], dma=possem)
            TWO_PI = 6.283185307179586
            C1 = 6.28125
            C2 = TWO_PI - C1
            PI = 3.141592653589793
            PIS = 3.1415925
            op("dve", "tensor_copy", out=RA[:], in_=POSI[:], r=[("POSI", 0)], w=[("RA", 0)])
            op("dve", "tensor_scalar", out=RB[:], in0=RA[:], scalar1=cs(C_INVF), scalar2=None, op0=ALU.mult,
               r=[("RA", 0), ("CST", 0)], w=[("RB", 0)])
            op("dve", "tensor_scalar", out=POSI[:], in0=RB[:], scalar1=1.0 / TWO_PI, scalar2=None, op0=ALU.mult,
               r=[("RB", 0)], w=[("POSI", 0)])
            op("dve", "tensor_copy", out=RA[:], in_=POSI[:], r=[("POSI", 0)], w=[("RA", 0)])
            op("dve", "scalar_tensor_tensor", out=RB[:], in0=RA[:], scalar=-C1, in1=RB[:], op0=ALU.mult, op1=ALU.add,
               r=[("RA", 0), ("RB", 0)], w=[("RB", 0)])
            op("dve", "scalar_tensor_tensor", out=RB[:], in0=RA[:], scalar=-C2, in1=RB[:], op0=ALU.mult, op1=ALU.add,
               r=[("RA", 0), ("RB", 0)], w=[("RB", 0)])
            op("dve", "tensor_scalar", out=RA[:], in0=RB[:], scalar1=-PIS, scalar2=PIS, op0=ALU.max, op1=ALU.min,
               r=[("RB", 0)], w=[("RA", 0)])
            op("act", "activation", out=SIN[:], in_=RA[:], func=AF.Sin, scale=cs(C_SGN),
               r=[("RA", 0), ("CST", 0)], w=[("SIN", 0)])
            op("dve", "tensor_scalar", out=TT[:], in0=RB[:], scalar1=PI / 2, scalar2=None, op0=ALU.add,
               r=[("RB", 0)], w=[("TT", 0)])
            op("dve", "tensor_scalar", out=HR[:], in0=TT[:], scalar1=PI, scalar2=-TWO_PI, op0=ALU.is_gt, op1=ALU.mult,
               r=[("TT", 0)], w=[("HR", 0)])
            op("dve", "tensor_tensor", out=TT[:], in0=TT[:], in1=HR[:], op=ALU.add,
               r=[("TT", 0), ("HR", 0)], w=[("TT", 0)])
            op("dve", "tensor_scalar", out=TT[:], in0=TT[:], scalar1=-PIS, scalar2=PIS, op0=ALU.max, op1=ALU.min,
               r=[("TT", 0)], w=[("TT", 0)])
            op("act", "activation", out=COS[:], in_=TT[:], func=AF.Sin, r=[("TT", 0)], w=[("COS", 0)])

        stats = {"bank": None}

        def stats_acc(kc):
            if kc == 0:
                b = nbank()
                reserved.add(b)
                stats["bank"] = b
            b = stats["bank"]
            sq = kc % 2
            op("act", "activation", out=SQ[:, sq, :], in_=X[:, kc, :], func=AF.Square,
               r=[("X", kc)], w=[("SQ", sq)])
            op("pe", "matmul", PS[:, b, :], lhsT=ones, rhs=SQ[:, sq, :], start=(kc == 0), stop=(kc == KC - 1),
               r=[("SQ", sq), ("CB", 0)], w=[PSp(b)])

        def stats_finish():
            b = stats["bank"]
            assert b is not None
            op("act", "activation", out=TT[:], in_=PS[:, b, :], func=AF.Ln, scale=1.0 / D, bias=cs(C_EPS),
               r=[PSp(b), ("CST", 0)], w=[("TT", 0)])
            op("act", "activation", out=RSTD[:], in_=TT[:], func=AF.Exp, scale=-0.5, r=[("TT", 0)], w=[("RSTD", 0)])
            reserved.discard(b)
            stats["bank"] = None

        def norm_to_h(l, kind):
            stats_finish()
            for kc in range(KC):
                op("dve", "scalar_tensor_tensor", out=Hh[:, kc, :], in0=X[:, kc, :], scalar=gcol(l, kind, kc),
                   in1=RSTD[:], op0=ALU.mult, op1=ALU.mult,
                   r=[("X", kc), ("RSTD", 0), ("GAINS", 0)], w=[("H", kc)])

        def proj_chunk(Wv, wbuf, j, b):
            for kc in range(KC):
                op("pe", "matmul", PS[:, b, :], lhsT=Wv[:, kc, j * 128:(j + 1) * 128], rhs=Hh[:, kc, :],
                   start=(kc == 0), stop=(kc == KC - 1), r=[("W", wbuf), ("H", kc)], w=[PSp(b)])

        def rope_chunk(b, dst_part_idx, qbi):
            op("act", "activation", out=QB[:, qbi, :], in_=PS[:, b, :], func=AF.Copy, r=[PSp(b)], w=[("QB", qbi)])
            b2 = nbank()
            op("pe", "matmul", PS[:, b2, :], lhsT=perm, rhs=QB[:, qbi, :], start=True, stop=True,
               r=[("QB", qbi), ("CB", 0)], w=[PSp(b2)])
            op("dve", "tensor_tensor", out=RA[:], in0=PS[:, b, :], in1=COS[:], op=ALU.mult,
               r=[PSp(b), ("COS", 0)], w=[("RA", 0)])
            op("dve", "tensor_tensor", out=RB[:], in0=PS[:, b2, :], in1=SIN[:], op=ALU.mult,
               r=[PSp(b2), ("SIN", 0)], w=[("RB", 0)])
            op("pool", "tensor_tensor", out=AR[:, dst_part_idx, :], in0=RA[:], in1=RB[:], op=ALU.add,
               r=[("RA", 0), ("RB", 0)], w=[ARp(dst_part_idx)])

        def mixer(t, l):
            norm_to_h(l, "mix")
            if l == 0:
                rope_tables(t)
            stage('norm')
            wi, wbuf = ring_next(l, "ux")
            Wv = WR[:, wbuf, 0:4096].rearrange("p (k n) -> p k n", k=KC)
            op("pool", "tensor_copy", out=U[:, :, 0:15], in_=HALO[:, l, :, :], r=[("HALO", l)], w=[("U", 0)])
            for c in range(2):
                b = nbank()
                proj_chunk(Wv, wbuf, c, b)
                op("dve", "tensor_copy", out=U[:, c, 15:15 + T], in_=PS[:, b, :], r=[PSp(b)], w=[("U", 0)])
            for c in range(2):
                b = nbank()
                proj_chunk(Wv, wbuf, 2 + c, b)
                op("act", "activation", out=AR[:, QX0 + c, :], in_=PS[:, b, :], func=AF.Copy,
                   r=[PSp(b)], w=[ARp(QX0 + c)])
            ring_done(wi)

            op("pool", "tensor_tensor", out=PB[:, :, 1:T + 15], in0=U[:, :, 1:T + 15], in1=U[:, :, 0:T + 14], op=ALU.add,
               r=[("U", 0)], w=[("PB", 0)])
            op("pool", "tensor_tensor", out=PC[64:128, 0, 3:T + 15], in0=PB[64:128, 0, 3:T + 15], in1=PB[64:128, 0, 1:T + 13],
               op=ALU.add, r=[("PB", 0)], w=[("PC", 0)])
            op("pool", "tensor_tensor", out=PC[:, 1, 3:T + 15], in0=PB[:, 1, 3:T + 15], in1=PB[:, 1, 1:T + 13],
               op=ALU.add, r=[("PB", 0)], w=[("PC", 0)])
            op("pool", "tensor_tensor", out=PB[:, 1, 7:T + 15], in0=PC[:, 1, 7:T + 15], in1=PC[:, 1, 3:T + 11],
               op=ALU.add, r=[("PC", 0)], w=[("PB", 0)])
            op("pool", "tensor_tensor", out=PC[64:128, 1, 15:T + 15], in0=PB[64:128, 1, 15:T + 15], in1=PB[64:128, 1, 7:T + 7],
               op=ALU.add, r=[("PB", 0)], w=[("PC", 0)])
            wins = [(PB, 0, 0), (PC, 0, 64), (PB, 1, 0), (PC, 1, 64)]
            for g, (buf, c, p0) in enumerate(wins):
                bn_ = "PB" if buf is PB else "PC"
                if t <= 1:
                    op("pool", "tensor_tensor", out=buf[p0:p0 + 64, c, 15:31], in0=buf[p0:p0 + 64, c, 15:31],
                       in1=CST[p0:p0 + 64, C_CORR + t * 32 + c * 16:C_CORR + t * 32 + c * 16 + 16], op=ALU.mult,
                       r=[(bn_, 0), ("CST", 0)], w=[(bn_, 0)])
                op("dve", "scalar_tensor_tensor", out=POOLED[p0:p0 + 64, c, :], in0=buf[p0:p0 + 64, c, 15:T + 15],
                   scalar=CST[p0:p0 + 64, C_INVW + c:C_INVW + c + 1], in1=U[p0:p0 + 64, c, 15:T + 15],
                   op0=ALU.mult, op1=ALU.subtract, r=[(bn_, 0), ("U", 0), ("CST", 0)], w=[("POOLED", c)])
            op("pool", "tensor_copy", out=HALO[:, l, :, :], in_=U[:, :, T:T + 15], r=[("U", 0)], w=[("HALO", l)])
            wi, wbuf = ring_next(l, "q")
            Wv = WR[:, wbuf, 0:4096].rearrange("p (k n) -> p k n", k=KC)
            for h in range(H):
                b = nbank()
                proj_chunk(Wv, wbuf, h, b)
                rope_chunk(b, QT0 + h, h % 2)
            ring_done(wi)
            stage('q')
            wi, wbuf = ring_next(l, "k")
            Wv = WR[:, wbuf, 0:4096].rearrange("p (k n) -> p k n", k=KC)
            for h in range(H):
                b = nbank()
                proj_chunk(Wv, wbuf, h, b)
                rope_chunk(b, KT0 + h, h % 2)
            ring_done(wi)
            stage('k')
            wi, wbuf = ring_next(l, "v")
            Wv = WR[:, wbuf, 0:4096].rearrange("p (k n) -> p k n", k=KC)
            for c in range(NCH):
                b = nbank()
                for kc in range(KC):
                    op("pe", "matmul", PS[:, b, :], lhsT=Hh[:, kc, c * 128:(c + 1) * 128], rhs=Wv[:, kc, :],
                       start=(kc == 0), stop=(kc == KC - 1), r=[("W", wbuf), ("H", kc)], w=[PSp(b)])
                op("act", "activation", out=AR[:, VT0 + c, :], in_=PS[:, b, :], func=AF.Copy,
                   r=[PSp(b)], w=[ARp(VT0 + c)])
            ring_done(wi)
            stage('v')
            wi, wbuf = ring_next(l, "gr")
            Wv = WR[:, wbuf, 0:4096].rearrange("p (k n) -> p k n", k=KC)
            for h in range(H):
                b = nbank()
                proj_chunk(Wv, wbuf, h, b)
                op("act", "activation", out=AR[:, SG0 + h, :], in_=PS[:, b, :], func=AF.Silu,
                   r=[PSp(b)], w=[ARp(SG0 + h)])
            ring_done(wi)
            stage('gr')
            stage('ux')
            maskT = CST[:, C_MASK:C_MASK + 512]
            ksc = CST[:, C_KSC:C_KSC + 512]
            gct = CST[:, C_GC:C_GC + 512]
            epsr = CST[:, C_EPSR:C_EPSR + 512]
            pre = {}
            for c in range(2):
                pre[c] = ring_next(l, f"mg{c}")
            gate_pieces = [(c, j) for c in range(2) for j in range(3)]

            def emit_gate_piece():
                if not gate_pieces:
                    return
                gc_, gj = gate_pieces.pop(0)
                gwi, gwbuf = pre[gc_]
                Wg_ = WR[:, gwbuf, 0:3072].rearrange("p (k n) -> p k n", k=KC)
                gb = nbank()
                proj_chunk(Wg_, gwbuf, gj, gb)
                op("act", "activation", out=TG[:, gc_ * 3 + gj, :], in_=PS[:, gb, :], func=AF.Copy,
                   r=[PSp(gb)], w=[("TG", gc_ * 3 + gj)])

            for c in range(NCH):
                cl = slice(c * 128, (c + 1) * 128)
                bs = nbank()
                for h in range(H):
                    op("pe", "matmul", PS[:, bs, h * 128:(h + 1) * 128], lhsT=AR[:, KT0 + h, cl], rhs=AR[:, QT0 + h, cl],
                       start=True, stop=True, r=[ARp(KT0 + h), ARp(QT0 + h)], w=[PSp(bs)])
                si = c % 2
                op("dve", "tensor_tensor", out=ST[:, si, :], in0=PS[:, bs, :], in1=maskT, op=ALU.mult,
                   r=[PSp(bs), ("CST", 0)], w=[("ST", si)])
                pb = nptb()
                for h in range(H):
                    op("pe", "transpose", PT[:, pb, h * 128:(h + 1) * 128], AR[:, KT0 + h, cl], ident,
                       r=[ARp(KT0 + h), ("CB", 0)], w=[("PT", pb)])
                op("dve", "tensor_tensor", out=KTOK[:, si, :], in0=PT[:, pb, :], in1=ksc, op=ALU.mult,
                   r=[("PT", pb), ("CST", 0)], w=[("KTOK", si)])
                bo = nbank()
                for h in range(H):
                    hs = slice(h * 128, (h + 1) * 128)
                    op("pe", "matmul", PS[:, bo, hs], lhsT=AR[:, VT0 + c, hs], rhs=ST[:, si, hs],
                       start=True, stop=False, r=[ARp(VT0 + c), ("ST", si)], w=[PSp(bo)])
                    op("pe", "matmul", PS[:, bo, hs], lhsT=RBF[:, l, hs], rhs=AR[:, QT0 + h, cl],
                       start=False, stop=True, r=[("RBF", l), ARp(QT0 + h)], w=[PSp(bo)])
                bk = nbank()
                for h in range(H):
                    hs = slice(h * 128, (h + 1) * 128)
                    op("pe", "matmul", PS[:, bk, hs], lhsT=KTOK[:, si, hs], rhs=AR[:, VT0 + c, hs],
                       start=True, stop=True, r=[("KTOK", si), ARp(VT0 + c)], w=[PSp(bk)])
                op("dve", "tensor_tensor", out=Y1[:], in0=R32[:, l, :], in1=PS[:, bk, :], op=ALU.add,
                   r=[("R32", l), PSp(bk)], w=[("Y1", 0)])
                op("pool", "tensor_tensor", out=RBF[:, l, :], in0=Y1[:], in1=gct, op=ALU.mult,
                   r=[("Y1", 0), ("CST", 0)], w=[("RBF", l)])
                op("pool", "tensor_tensor", out=R32[:, l, :], in0=Y1[:], in1=gct, op=ALU.mult,
                   r=[("Y1", 0), ("CST", 0)], w=[("R32", l)])
                op("act", "activation", out=YSQ[:, si, :], in_=PS[:, bo, :], func=AF.Square,
                   r=[PSp(bo)], w=[("YSQ", si)])
                emit_gate_piece()
                bn = nbank()
                op("pe", "matmul", PS[:, bn, :], lhsT=ones, rhs=YSQ[:, si, :], start=True, stop=True,
                   r=[("YSQ", si), ("CB", 0)], w=[PSp(bn)])
                op("dve", "scalar_tensor_tensor", out=TT[:], in0=PS[:, bn, :], scalar=1.0 / 128, in1=epsr,
                   op0=ALU.mult, op1=ALU.add, r=[PSp(bn), ("CST", 0)], w=[("TT", 0)])
                op("act", "activation", out=TT[:], in_=TT[:], func=AF.Ln, r=[("TT", 0)], w=[("TT", 0)])
                op("act", "activation", out=HR[:], in_=TT[:], func=AF.Exp, scale=-0.5, r=[("TT", 0)], w=[("HR", 0)])
                emit_gate_piece()
                op("dve", "tensor_tensor", out=RA[:], in0=PS[:, bo, :], in1=HR[:], op=ALU.mult,
                   r=[PSp(bo), ("HR", 0)], w=[("RA", 0)])
                op("pool", "tensor_tensor", out=AR[:, YR0:YR0 + 4, cl],
                   in0=RA[:].rearrange("p (h i) -> p h i", h=H), in1=AR[:, SG0:SG0 + 4, cl], op=ALU.mult,
                   r=[("RA", 0)] + [ARp(SG0 + h) for h in range(H)], w=[ARp(YR0 + h) for h in range(H)])

            stage('ret')
            for c in range(2):
                b = nbank()
                op("pe", "matmul", PS[:, b, :], lhsT=WMIX[:, (l * 2 + c) * 128:(l * 2 + c + 1) * 128], rhs=POOLED[:, c, :],
                   start=True, stop=True, r=[("WMIX", 0), ("POOLED", c)], w=[PSp(b)])
                op("act", "activation", out=YPOOL[:, c, :], in_=PS[:, b, :], func=AF.Copy, scale=gcol(l, "ps", c),
                   r=[PSp(b), ("GAINS", 0)], w=[("YPOOL", c)])

            stage('pool')
            for hc in range(2):
                ba = nbank()
                bd = nbank()
                for hh in range(2):
                    h = 2 * hc + hh
                    r0 = hh * 64
                    eb = h % 2
                    bx = [nbank(), nbank()]
                    for mc in range(2):
                        op("pe", "matmul", PS[:, bx[mc], :], lhsT=MK[r0:r0 + 64, l, hc, mc * 128:(mc + 1) * 128],
                           rhs=AR[r0:r0 + 64, QX0 + hc, :], start=True, stop=True,
                           r=[("MK", l), ARp(QX0 + hc)], w=[PSp(bx[mc])])
                    for mc in range(2):
                        op("act", "activation", out=EXPT[:, eb, mc, :], in_=PS[:, bx[mc], :], func=AF.Exp, scale=0.125,
                           r=[PSp(bx[mc])], w=[("EXPT", eb)])
                    for mc in range(2):
                        first = (hh == 0 and mc == 0)
                        last = (hh == 1 and mc == 1)
                        op("pe", "matmul", PS[:, ba, :], lhsT=MV[:, l, mc, h, :], rhs=EXPT[:, eb, mc, :],
                           start=first, stop=last, r=[("MV", l), ("EXPT", eb)], w=[PSp(ba)])
                        op("pe", "matmul", PS[:, bd, :], lhsT=CB[:, B_OPAD + hh * 128:B_OPAD + (hh + 1) * 128],
                           rhs=EXPT[:, eb, mc, :], start=first, stop=last, r=[("CB", 0), ("EXPT", eb)], w=[PSp(bd)])
                op("dve", "reciprocal", out=RDEN[:], in_=PS[:, bd, :], r=[PSp(bd)], w=[("RDEN", 0)])
                op("dve", "tensor_tensor", out=YX[:, hc, :], in0=PS[:, ba, :], in1=RDEN[:], op=ALU.mult,
                   r=[PSp(ba), ("RDEN", 0)], w=[("YX", hc)])

            stage('xattn')
            assert not gate_pieces
            for c in range(8):
                wi, wbuf = pre[c] if c < 2 else ring_next(l, f"mg{c}")
                Wg = WR[:, wbuf, 0:3072].rearrange("p (k n) -> p k n", k=KC)
                Wr = WR[:, wbuf, 3072:3584].rearrange("p (k n) -> p k n", k=4)
                Wp = WR[:, wbuf, 3584:3840].rearrange("p (k n) -> p k n", k=2)
                Wx = WR[:, wbuf, 3840:4096].rearrange("p (k n) -> p k n", k=2)
                ts = (c % 2) * 3
                bg = []
                for j in range(3):
                    if c < 2:
                        op("act", "activation", out=TG[:, ts + j, :], in_=TG[:, ts + j, :], func=AF.Tanh, scale=0.5,
                           r=[("TG", ts + j)], w=[("TG", ts + j)])
                        continue
                    b = nbank()
                    bg.append(b)
                    proj_chunk(Wg, wbuf, j, b)
                    op("act", "activation", out=TG[:, ts + j, :], in_=PS[:, b, :], func=AF.Tanh, scale=0.5,
                       r=[PSp(b)], w=[("TG", ts + j)])
                br = nbank()
                for h in range(H):
                    op("pe", "matmul", PS[:, br, :], lhsT=Wr[:, h, :], rhs=AR[:, YR0 + h, :], start=(h == 0), stop=(h == H - 1),
                       r=[("W", wbuf), ARp(YR0 + h)], w=[PSp(br)])
                bp = nbank()
                for j in range(2):
                    op("pe", "matmul", PS[:, bp, :], lhsT=Wp[:, j, :], rhs=YPOOL[:, j, :], start=(j == 0), stop=(j == 1),
                       r=[("W", wbuf), ("YPOOL", j)], w=[PSp(bp)])
                bxx = nbank()
                for j in range(2):
                    op("pe", "matmul", PS[:, bxx, :], lhsT=Wx[:, j, :], rhs=YX[:, j, :], start=(j == 0), stop=(j == 1),
                       r=[("W", wbuf), ("YX", j)], w=[PSp(bxx)])
                ring_done(wi)
                for j, bb in enumerate((br, bp, bxx)):
                    op("dve", "scalar_tensor_tensor", out=Mm[:, ts + j, :], in0=TG[:, ts + j, :], scalar=1.0, in1=PS[:, bb, :],
                       op0=ALU.add, op1=ALU.mult, r=[("TG", ts + j), PSp(bb)], w=[("Mm", ts + j)])
                op("pool", "tensor_tensor", out=Mm[:, ts, :], in0=Mm[:, ts, :], in1=Mm[:, ts + 1, :], op=ALU.add,
                   r=[("Mm", ts), ("Mm", ts + 1)], w=[("Mm", ts)])
                op("pool", "tensor_tensor", out=AR[:, c, :], in0=Mm[:, ts, :], in1=Mm[:, ts + 2, :], op=ALU.add,
                   r=[("Mm", ts), ("Mm", ts + 2)], w=[ARp(c)])

            stage('merge')
            for s in range(2):
                wi, wbuf = ring_next(l, f"wo{s}")
                Wv = WR[:, wbuf, 0:4096].rearrange("p (k n) -> p k n", k=KC)
                for j in range(4):
                    oc = s * 4 + j
                    b = nbank()
                    for kc in range(KC):
                        op("pe", "matmul", PS[:, b, :], lhsT=Wv[:, kc, j * 128:(j + 1) * 128], rhs=AR[:, kc, :],
                           start=(kc == 0), stop=(kc == KC - 1), r=[("W", wbuf), ARp(kc)], w=[PSp(b)])
                    op("dve", "scalar_tensor_tensor", out=X[:, oc, :], in0=PS[:, b, :], scalar=0.5, in1=X[:, oc, :],
                       op0=ALU.mult, op1=ALU.add, r=[PSp(b), ("X", oc)], w=[("X", oc)])
                    if oc >= 1:
                        stats_acc(oc - 1)
                ring_done(wi)
            stats_acc(KC - 1)

        def ffn(t, l):
            norm_to_h(l, "ffn")
            for s in range(11):
                wi, wbuf = ring_next(l, f"fi{s}")
                Wv = WR[:, wbuf, 0:4096].rearrange("p (k n) -> p k n", k=KC)
                for j in range(2):
                    c = 2 * s + j
                    ba = nbank()
                    proj_chunk(Wv, wbuf, j, ba)
                    bb = nbank()
                    proj_chunk(Wv, wbuf, 2 + j, bb)
                    ti = c % 2
                    op("act", "activation", out=TH[:, ti, :], in_=PS[:, ba, :], func=AF.Silu,
                       r=[PSp(ba)], w=[("TH", ti)])
                    op("dve", "tensor_tensor", out=AR[:, c, :], in0=TH[:, ti, :], in1=PS[:, bb, :], op=ALU.mult,
                       r=[("TH", ti), PSp(bb)], w=[ARp(c)])
                ring_done(wi)
            for oc in range(8):
                wi, wbuf = ring_next(l, f"fo{oc}")
                Wv = WR[:, wbuf, 0:2816].rearrange("p (k n) -> p k n", k=FKC)
                b = nbank()
                for kc in range(FKC):
                    op("pe", "matmul", PS[:, b, :], lhsT=Wv[:, kc, :], rhs=AR[:, kc, :],
                       start=(kc == 0), stop=(kc == FKC - 1), r=[("W", wbuf), ARp(kc)], w=[PSp(b)])
                op("dve", "tensor_tensor", out=X[:, oc, :], in0=PS[:, b, :], in1=X[:, oc, :], op=ALU.add,
                   r=[PSp(b), ("X", oc)], w=[("X", oc)])
                if oc >= 1:
                    stats_acc(oc - 1)
                if (oc % 4 == 3) and l == DEPTH - 1 and t < NT - 1:
                    hx = oc // 4
                    op("sp", "dma_start", out=snd_d[hx].rearrange("p (k n) -> p k n", k=4), in_=X[:, 4 * hx:4 * hx + 4, :],
                       r=[("X", kc) for kc in range(4 * hx, 4 * hx + 4)], w=[("SND", hx)], dma=sndsem[hx])
                    op("pool", "collective_compute", "AllGather", ALU.bypass, replica_groups=groups,
                       ins=[snd_d[hx]], outs=[rcv_d[hx]], r=[("SND", hx)], w=[("RCV", hx)], dma=ccsem[hx], inc=1)
                ring_done(wi)
            stats_acc(KC - 1)

        xT_v = xT_d.rearrange("(kc p) s -> p kc s", p=128)
        out_v = out_d.rearrange("(kc p) s -> p kc s", p=128)
        last_store = None
        Xparts = [("X", kc) for kc in range(KC)]
        for t in range(NT):
            op("sp", "dma_start", out=X[:], in_=xT_v[:, :, t * T:(t + 1) * T], w=Xparts, dma=xsem)
            if t >= 1:
                for kc in range(KC):
                    si = kc % 4
                    op("sp", "dma_start", out=STG[:, si, :], in_=rcv_d[kc // 4][0:128, (kc % 4) * T:(kc % 4 + 1) * T],
                       r=[("RCV", kc // 4)], w=[("STG", si)], dma=stgsem[si])
                    op("dve", "scalar_tensor_tensor", out=X[:, kc, :], in0=STG[:, si, :], scalar=cs(C_SG),
                       in1=X[:, kc, :], op0=ALU.mult, op1=ALU.add,
                       r=[("STG", si), ("X", kc), ("CST", 0)], w=[("X", kc)])
            for kc in range(KC):
                stats_acc(kc)
            for l in range(DEPTH):
                mixer(t, l)
                ffn(t, l)
            if t == 0:
                op("pool", "tensor_scalar", out=R32[:], in0=R32[:], scalar1=cs(C_KEEP), scalar2=None, op0=ALU.mult,
                   r=[("R32", l) for l in range(DEPTH)] + [("CST", 0)], w=[("R32", l) for l in range(DEPTH)])
                op("pool", "tensor_scalar", out=RBF[:], in0=RBF[:], scalar1=cs(C_KEEP), scalar2=None, op0=ALU.mult,
                   r=[("RBF", l) for l in range(DEPTH)] + [("CST", 0)], w=[("RBF", l) for l in range(DEPTH)])
                op("pool", "tensor_scalar", out=HALO[:], in0=HALO[:], scalar1=cs(C_KEEP), scalar2=None, op0=ALU.mult,
                   r=[("HALO", l) for l in range(DEPTH)] + [("CST", 0)], w=[("HALO", l) for l in range(DEPTH)])
            stats_finish()
            for kc in range(KC):
                op("dve", "scalar_tensor_tensor", out=X[:, kc, :], in0=X[:, kc, :], scalar=gfin(kc),
                   in1=RSTD[:], op0=ALU.mult, op1=ALU.mult,
                   r=[("X", kc), ("RSTD", 0), ("GAINS", 0)], w=[("X", kc)])
            last_store = op("sp", "dma_start", out=out_v[:, :, t * T:(t + 1) * T], in_=X[:], r=Xparts, dma=osem)
        if stopped[0]:
            stopped[0] = False
            last_store = op("sp", "dma_start", out=out_v[:, :, 0:T], in_=X[:],
                            r=[("X", kc) for kc in range(KC)] + [("RSTD", 0)], dma=osem)
        else:
            assert ring["cur"] == len(slab_list)

        block = es.enter_context(nc.Block())

        @block.sync
        def _(e):
            sc.emit("sp", e, esem)
            e.wait_ge(osem, last_store.val)

        @block.tensor
        def _(e):
            sc.emit("pe", e, esem)

        @block.scalar
        def _(e):
            sc.emit("act", e, esem)

        @block.vector
        def _(e):
            sc.emit("dve", e, esem)

        @block.gpsimd
        def _(e):
            sc.emit("pool", e, esem)

    return nc


def _slab(Wm, cols):
    K = Wm.shape[0]
    sub = Wm[:, cols]
    return np.ascontiguousarray(sub.reshape(K // 128, 128, -1).transpose(1, 0, 2)).reshape(128, -1)


def _layer_line(l, w_in, w_up_ret, w_up_pool, w_up_x, w_out, w_ffn_in, w_ffn_out, w_mem_kv):
    ar = np.arange
    parts = {}
    parts["memkv"] = _slab(w_mem_kv[l], ar(512))
    parts["q"] = _slab(w_in[l], ar(0, 512))
    parts["k"] = _slab(w_in[l], ar(512, 1024))
    parts["v"] = _slab(w_in[l], ar(1024, 1536))
    parts["gr"] = _slab(w_in[l], ar(1536, 2048))
    parts["ux"] = _slab(w_in[l], ar(2048, 2560))
    for c in range(8):
        gcols = np.concatenate([2560 + j * 1024 + c * 128 + ar(128) for j in range(3)])
        oc = c * 128 + ar(128)
        parts[f"mg{c}"] = np.concatenate(
            [_slab(w_in[l], gcols), _slab(w_up_ret[l], oc), _slab(w_up_pool[l], oc), _slab(w_up_x[l], oc)], axis=1)
    for s in range(2):
        parts[f"wo{s}"] = _slab(w_out[l], ar(s * 512, (s + 1) * 512))
    for s in range(11):
        cols = np.concatenate([(2 * s) * 128 + ar(128), (2 * s + 1) * 128 + ar(128),
                               FH + (2 * s) * 128 + ar(128), FH + (2 * s + 1) * 128 + ar(128)])
        parts[f"fi{s}"] = _slab(w_ffn_in[l], cols)
    for j in range(8):
        parts[f"fo{j}"] = _slab(w_ffn_out[l], ar(j * 128, (j + 1) * 128))
    line = np.concatenate([parts[n] for n, _ in SLABS], axis=1)
    assert line.shape == (128, LINE), line.shape
    return line


def _consts(stage):
    c = np.zeros((128, NCONST), np.float64)
    g = 1.0 - np.exp2(-5.0 - np.arange(H))
    j = np.arange(128)
    dk = 128.0 ** -0.5
    for h in range(H):
        ginv = g[h] ** (-(j + 1.0))
        m = (j[:, None] <= j[None, :]) * (ginv[:, None] * dk)
        c[:, C_MASK + h * 128:C_MASK + (h + 1) * 128] = m
        c[:, C_KSC + h * 128:C_KSC + (h + 1) * 128] = (ginv * dk)[:, None]
        c[:, C_GC + h * 128:C_GC + (h + 1) * 128] = g[h] ** 128.0
        c[:, C_EPSR + h * 128:C_EPSR + (h + 1) * 128] = (EPS * g[h] ** (-2.0 * (j + 1.0)))[None, :]
    wins = (2, 4, 8, 16)
    for ch in range(2):
        for half in range(2):
            w = wins[ch * 2 + half]
            p = slice(half * 64, half * 64 + 64)
            c[p, C_INVW + ch] = 1.0 / w
            tt = np.arange(16)
            real = (w / np.minimum(tt + 1, w))[None, :]
            for st in range(2):
                val = real if st == stage else 1.0
                c[p, C_CORR + st * 32 + ch * 16:C_CORR + st * 32 + ch * 16 + 16] = val
    inv_freq = (10000.0 ** (-np.arange(0, 128, 2, dtype=np.float32) / np.float32(128))).astype(np.float32)
    c[:, C_INVF] = np.concatenate([inv_freq, inv_freq])
    c[0:64, C_SGN] = -1.0
    c[64:128, C_SGN] = 1.0
    c[:, C_EPS] = EPS
    c[:, C_SG] = float(stage)
    c[:, C_KEEP] = 1.0 - float(stage)
    cb = np.zeros((128, NCB), np.float32)
    cb[:, B_ID:B_ID + 128] = np.eye(128)
    pm = np.zeros((128, 128), np.float32)
    mm = np.arange(128)
    pm[(mm + 64) % 128, mm] = 1.0
    cb[:, B_PERM:B_PERM + 128] = pm
    cb[:, B_ONES:B_ONES + 128] = 1.0
    cb[:, B_OPAD + 0:B_OPAD + 64] = 1.0
    cb[:, B_OPAD + 128 + 64:B_OPAD + 256] = 1.0
    return c.astype(np.float32), cb


_PROG_CACHE = {}


def kernel(x, mem, positions, norm_mix, w_in, w_up_ret, w_pool_mix, pool_scale, w_up_pool,
           norm_mem, w_mem_kv, w_up_x, w_out, norm_ffn, w_ffn_in, w_ffn_out, final_norm):
    x = np.asarray(x, np.float32)
    mem = np.asarray(mem, np.float32)
    B, S, _ = x.shape
    DEPTH = int(np.asarray(norm_mix).shape[0])
    f = lambda a: np.asarray(a, np.float32)
    w_in, w_up_ret, w_up_pool, w_up_x, w_out = f(w_in), f(w_up_ret), f(w_up_pool), f(w_up_x), f(w_out)
    w_ffn_in, w_ffn_out, w_mem_kv, w_pool_mix = f(w_ffn_in), f(w_ffn_out), f(w_mem_kv), f(w_pool_mix)
    norm_mix, norm_ffn, norm_mem, pool_scale, final_norm = f(norm_mix), f(norm_ffn), f(norm_mem), f(pool_scale), f(final_norm)

    LD = DEPTH // 2
    NT = S // T
    SP_ = (NT + 1) * T
    lines = [_layer_line(l, w_in, w_up_ret, w_up_pool, w_up_x, w_out, w_ffn_in, w_ffn_out, w_mem_kv)
             for l in range(DEPTH)]
    pos = np.asarray(positions, np.int32).reshape(S)

    def stage_inputs(stage):
        ls = list(range(stage * LD, (stage + 1) * LD))
        wf = np.stack([lines[l] for l in ls], axis=0)
        cst, cstb = _consts(stage)
        gains = np.zeros((128, LD * 26 + 8), np.float32)
        wmix = np.zeros((128, LD, 2, 128), np.float32)
        for i, l in enumerate(ls):
            gains[:, i * 26 + 0:i * 26 + 8] = norm_mix[l].reshape(8, 128).T
            gains[:, i * 26 + 8:i * 26 + 16] = norm_ffn[l].reshape(8, 128).T
            gains[:, i * 26 + 16:i * 26 + 24] = norm_mem[l].reshape(8, 128).T
            gains[:, i * 26 + 24:i * 26 + 26] = pool_scale[l].reshape(2, 128).T
            for g in range(4):
                c, half = g // 2, g % 2
                wmix[half * 64:half * 64 + 64, i, c, half * 64:half * 64 + 64] = w_pool_mix[l, g]
        gains[:, LD * 26:LD * 26 + 8] = final_norm.reshape(8, 128).T
        posp = np.zeros((1, SP_), np.int32)
        posp[0, stage * T:stage * T + S] = pos
        return dict(wf=wf, cst=cst, cstb=cstb, gains=gains, wmix=wmix.reshape(128, -1), pos=posp)

    st_in = [stage_inputs(0), stage_inputs(1)]
    key = (S, LD, B)
    if key not in _PROG_CACHE:
        _PROG_CACHE[key] = build_program(S, LD, B)
    nc = _PROG_CACHE[key]
    in_maps = []
    for b in range(B):
        memT = np.ascontiguousarray(mem[b].T)
        xa = np.zeros((D, SP_), np.float32)
        xa[:, 0:S] = x[b].T
        in_maps.append(dict(st_in[0], xT=xa, memT=memT))
        in_maps.append(dict(st_in[1], xT=np.zeros((D, SP_), np.float32), memT=memT))
    ncores = 2 * B
    res = run_bass_kernel_spmd(nc, in_maps, core_ids=list(range(ncores)))
    out = np.stack([np.ascontiguousarray(res.results[2 * b + 1]["outT"][:, T:T + S].T) for b in range(B)], axis=0)
    return out.astype(np.float32)
```
